# Optimizing a Trainium2 kernel written in Bass

```python
import math
import jax, jax.numpy as jnp
from jax import lax
import numpy as np


D_MODEL = 1024
BATCH = 4
SEQ = 8192
DEPTH = 2

CHUNK = 64
Q_BLOCK = 128
HEAD_DIM = 64
N_BRANCHES = 4
BRANCH_WIDTH = 4 * HEAD_DIM

DIFF_HEADS = 4
DIFF_DH = HEAD_DIM // 2

CHUNK_HEADS = 4
BAND_PREV = 8
REL_CLIP = 128

MLA_HEADS = 4
MLA_Q_LORA = D_MODEL // 4
MLA_KV_LORA = D_MODEL // 8
MLA_NOPE = HEAD_DIM
MLA_ROPE = HEAD_DIM // 2
MLA_V = HEAD_DIM
ROPE_BASE = 10000.0

FOX_HEADS = 4

T5_BUCKETS = 32
T5_MAX_DIST = 128

N_GROUPS = 4
EXPERTS_PER_GROUP = 8
N_EXPERTS = N_GROUPS * EXPERTS_PER_GROUP
TOP_K = 2
D_EXPERT = D_MODEL // 2
MOE_BLOCK = 256

DEEPNORM_ALPHA = (2 * DEPTH) ** 0.25
DEEPNORM_BETA = (8 * DEPTH) ** -0.25
NORM_EPS = 1e-5
NEG_INF = -1e30

IN_SPLITS = (
    DIFF_HEADS * HEAD_DIM, DIFF_HEADS * HEAD_DIM, DIFF_HEADS * HEAD_DIM,
    CHUNK_HEADS * HEAD_DIM, CHUNK_HEADS * HEAD_DIM, CHUNK_HEADS * HEAD_DIM,
    MLA_Q_LORA, MLA_KV_LORA, MLA_ROPE,
    FOX_HEADS * HEAD_DIM, FOX_HEADS * HEAD_DIM, FOX_HEADS * HEAD_DIM, FOX_HEADS,
)
IN_OFFSETS = tuple(int(o) for o in np.cumsum(IN_SPLITS)[:-1])
D_IN = sum(IN_SPLITS)

kernel_name = 'hybrid_chunk_causal_moe_trunk'


def layer_norm(x, g, b):
    xf = x.astype(jnp.float32)
    mu = jnp.mean(xf, axis=-1, keepdims=True)
    var = jnp.mean(jnp.square(xf - mu), axis=-1, keepdims=True)
    return ((xf - mu) * lax.rsqrt(var + NORM_EPS)).astype(x.dtype) * g + b


def rms_norm(x, g):
    xf = x.astype(jnp.float32)
    return (xf * lax.rsqrt(jnp.mean(xf * xf, axis=-1, keepdims=True) + NORM_EPS)).astype(x.dtype) * g


def apply_rope(x, cos, sin):
    x1, x2 = jnp.split(x, 2, axis=-1)
    c = cos[:, None, :].astype(x.dtype)
    s = sin[:, None, :].astype(x.dtype)
    return jnp.concatenate([x1 * c - x2 * s, x1 * s + x2 * c], axis=-1)


def chunk_causal_mask(q_pos, k_pos):
    return (k_pos[None, :] // CHUNK) <= (q_pos[:, None] // CHUNK)


def t5_bucket(rel):
    nb = T5_BUCKETS // 2
    max_exact = nb // 2
    ret = jnp.where(rel > 0, nb, 0)
    n = jnp.abs(rel)
    nf = jnp.maximum(n, 1).astype(jnp.float32)
    large = max_exact + (jnp.log(nf / max_exact) / math.log(T5_MAX_DIST / max_exact) * (nb - max_exact)).astype(jnp.int32)
    large = jnp.minimum(large, nb - 1)
    return ret + jnp.where(n < max_exact, n, large)


def t5_bias(q_pos, k_pos, table):
    rel = k_pos[None, :] - q_pos[:, None]
    return jnp.moveaxis(table[t5_bucket(rel)], -1, 0).astype(jnp.float32)


def sweep_query_blocks(block_fn, *q_side):
    b, s = q_side[0].shape[:2]
    n_blk = s // Q_BLOCK
    def split(a):
        return jnp.moveaxis(a.reshape((b, n_blk, Q_BLOCK) + a.shape[2:]), 1, 0)
    out = lax.map(lambda args: block_fn(*args), (jnp.arange(n_blk),) + tuple(split(a) for a in q_side))
    out = jnp.moveaxis(out, 0, 1)
    return out.reshape((b, s) + out.shape[3:])


def differential_attention(q, k, v, lam, norm_g, t5_table, layer_idx):
    b, s = q.shape[:2]
    lambda_init = 0.8 - 0.6 * math.exp(-0.3 * layer_idx)
    lamf = lam.astype(jnp.float32)
    lam_full = jnp.exp(jnp.sum(lamf[0] * lamf[1])) - jnp.exp(jnp.sum(lamf[2] * lamf[3])) + lambda_init
    k_pos = jnp.arange(s)
    scale = DIFF_DH ** -0.5

    def block(blk, qb):
        q_pos = blk * Q_BLOCK + jnp.arange(Q_BLOCK)
        logits = jnp.einsum('bqhcd,bkhcd->bchqk', qb, k, preferred_element_type=jnp.float32) * scale
        logits = logits + t5_bias(q_pos, k_pos, t5_table)
        logits = jnp.where(chunk_causal_mask(q_pos, k_pos), logits, NEG_INF)
        p = jax.nn.softmax(logits, axis=-1)
        attn = (p[:, 0] - lam_full * p[:, 1]).astype(v.dtype)
        return jnp.einsum('bhqk,bkhe->bqhe', attn, v)

    o = sweep_query_blocks(block, q)
    o = rms_norm(o, norm_g) * (1.0 - lambda_init)
    return o.reshape(b, s, -1)


def chunk_band_attention(q, k, v, rel_table):
    b, s, h, dh = q.shape
    nc = s // CHUNK
    band = (BAND_PREV + 1) * CHUNK
    qc = q.reshape(b, nc, CHUNK, h, dh)

    def gather_band(a):
        ac = jnp.pad(a.reshape(b, nc, CHUNK, h, dh), ((0, 0), (BAND_PREV, 0), (0, 0), (0, 0), (0, 0)))
        return jnp.concatenate([ac[:, j:j + nc] for j in range(BAND_PREV + 1)], axis=2)

    kb, vb = gather_band(k), gather_band(v)
    logits = jnp.einsum('bnqhd,bnkhd->bnhqk', qc, kb, preferred_element_type=jnp.float32) * dh ** -0.5
    key_off = jnp.arange(band) - BAND_PREV * CHUNK
    rel = key_off[None, :] - jnp.arange(CHUNK)[:, None]
    bias = rel_table[jnp.clip(rel, -REL_CLIP, REL_CLIP) + REL_CLIP]
    logits = logits + jnp.moveaxis(bias, -1, 0).astype(jnp.float32)
    key_chunk = jnp.arange(nc)[:, None] + jnp.arange(BAND_PREV + 1)[None, :] - BAND_PREV
    valid = jnp.repeat(key_chunk >= 0, CHUNK, axis=1)
    logits = jnp.where(valid[None, :, None, None, :], logits, NEG_INF)
    p = jax.nn.softmax(logits, axis=-1).astype(v.dtype)
    o = jnp.einsum('bnhqk,bnkhd->bnqhd', p, vb)
    return o.reshape(b, s, h * dh)


def latent_attention(c_q, c_kv, k_rope, q_norm_g, kv_norm_g, w_uq, w_ukv, cos, sin):
    b, s, _ = c_q.shape
    q = (rms_norm(c_q, q_norm_g) @ w_uq).reshape(b, s, MLA_HEADS, MLA_NOPE + MLA_ROPE)
    kv = (rms_norm(c_kv, kv_norm_g) @ w_ukv).reshape(b, s, MLA_HEADS, MLA_NOPE + MLA_V)
    q = jnp.concatenate([q[..., :MLA_NOPE], apply_rope(q[..., MLA_NOPE:], cos, sin)], axis=-1)
    k_r = jnp.broadcast_to(apply_rope(k_rope[:, :, None, :], cos, sin), (b, s, MLA_HEADS, MLA_ROPE))
    k = jnp.concatenate([kv[..., :MLA_NOPE], k_r], axis=-1)
    v = kv[..., MLA_NOPE:]
    k_pos = jnp.arange(s)
    scale = (MLA_NOPE + MLA_ROPE) ** -0.5

    def block(blk, qb):
        q_pos = blk * Q_BLOCK + jnp.arange(Q_BLOCK)
        logits = jnp.einsum('bqhd,bkhd->bhqk', qb, k, preferred_element_type=jnp.float32) * scale
        logits = jnp.where(chunk_causal_mask(q_pos, k_pos), logits, NEG_INF)
        p = jax.nn.softmax(logits, axis=-1).astype(v.dtype)
        return jnp.einsum('bhqk,bkhe->bqhe', p, v)

    return sweep_query_blocks(block, q).reshape(b, s, -1)


def forgetting_attention(q, k, v, f_logit):
    b, s, h, dh = q.shape
    cum = jnp.cumsum(jax.nn.log_sigmoid(f_logit.astype(jnp.float32)), axis=1)
    cum_k = jnp.moveaxis(cum, 1, 2)
    k_pos = jnp.arange(s)
    scale = dh ** -0.5

    def block(blk, qb, cum_q):
        q_pos = blk * Q_BLOCK + jnp.arange(Q_BLOCK)
        logits = jnp.einsum('bqhd,bkhd->bhqk', qb, k, preferred_element_type=jnp.float32) * scale
        logits = logits + jnp.moveaxis(cum_q, 1, 2)[..., None] - cum_k[:, :, None, :]
        logits = jnp.where(k_pos[None, :] <= q_pos[:, None], logits, NEG_INF)
        p = jax.nn.softmax(logits, axis=-1).astype(v.dtype)
        return jnp.einsum('bhqk,bkhe->bqhe', p, v)

    return sweep_query_blocks(block, q, cum).reshape(b, s, -1)


def mixing_sublayer(h, layer_idx, t5_table, cos, sin, w_in, b_forget, diff_lambda, diff_norm_g,
                    chunk_rel_bias, mla_q_norm_g, mla_kv_norm_g, mla_w_uq, mla_w_ukv,
                    w_gate, b_gate, w_branch, w_out):
    b, s, _ = h.shape
    (dq, dk, dv, cq, ck, cv, mq, mkv, mkr, fq, fk, fv, ff) = jnp.split(h @ w_in, IN_OFFSETS, axis=-1)
    o_diff = differential_attention(dq.reshape(b, s, DIFF_HEADS, 2, DIFF_DH), dk.reshape(b, s, DIFF_HEADS, 2, DIFF_DH),
                                    dv.reshape(b, s, DIFF_HEADS, 2 * DIFF_DH), diff_lambda, diff_norm_g,
                                    t5_table, layer_idx)
    o_chunk = chunk_band_attention(cq.reshape(b, s, CHUNK_HEADS, HEAD_DIM), ck.reshape(b, s, CHUNK_HEADS, HEAD_DIM),
                                   cv.reshape(b, s, CHUNK_HEADS, HEAD_DIM), chunk_rel_bias)
    o_mla = latent_attention(mq, mkv, mkr, mla_q_norm_g, mla_kv_norm_g, mla_w_uq, mla_w_ukv, cos, sin)
    o_fox = forgetting_attention(fq.reshape(b, s, FOX_HEADS, HEAD_DIM), fk.reshape(b, s, FOX_HEADS, HEAD_DIM),
                                 fv.reshape(b, s, FOX_HEADS, HEAD_DIM), ff + b_forget)
    merged = jnp.zeros_like(h)
    for n, o_n in enumerate((o_diff, o_chunk, o_mla, o_fox)):
        gate = jax.nn.sigmoid(h @ w_gate[n] + b_gate[n])
        merged = merged + gate * (o_n @ w_branch[n])
    return merged @ w_out


def hierarchical_moe(x, wg, bg, we, be, w1, w3, w2):
    b, s, d = x.shape
    n_tok = b * s
    xt = x.reshape(n_tok, d)
    g_prob = jax.nn.softmax((xt @ wg).astype(jnp.float32) + bg.astype(jnp.float32), axis=-1)
    g_val, g_idx = lax.top_k(g_prob, 1)
    e_logits = ((xt @ we).astype(jnp.float32) + be.astype(jnp.float32)).reshape(n_tok, N_GROUPS, EXPERTS_PER_GROUP)
    e_prob = jax.nn.softmax(e_logits[jnp.arange(n_tok), g_idx[:, 0]], axis=-1)
    e_val, e_loc = lax.top_k(e_prob, TOP_K)
    gate = g_val * e_val / jnp.sum(e_val, axis=-1, keepdims=True)
    expert = (g_idx * EXPERTS_PER_GROUP + e_loc).reshape(-1)
    n_slots = n_tok * TOP_K
    token = jnp.arange(n_slots) // TOP_K
    onehot = jax.nn.one_hot(expert, N_EXPERTS, dtype=jnp.int32)
    rank = jnp.cumsum(onehot, axis=0)[jnp.arange(n_slots), expert] - 1
    counts = jnp.sum(onehot, axis=0)
    padded = (counts + MOE_BLOCK - 1) // MOE_BLOCK * MOE_BLOCK
    pad_end = jnp.cumsum(padded)
    dest = (pad_end - padded)[expert] + rank
    n_blocks = -(-n_slots // MOE_BLOCK) + N_EXPERTS
    tok_buf = jnp.zeros((n_blocks * MOE_BLOCK,), jnp.int32).at[dest].set(token)
    gate_buf = jnp.zeros((n_blocks * MOE_BLOCK,), jnp.float32).at[dest].set(gate.reshape(-1))
    blk_start = jnp.arange(n_blocks) * MOE_BLOCK
    blk_expert = jnp.minimum(jnp.sum(pad_end[None, :] <= blk_start[:, None], axis=1), N_EXPERTS - 1)

    def run_block(args):
        e, idx, g = args
        xb = xt[idx]
        hb = jax.nn.silu(xb @ w1[e]) * (xb @ w3[e])
        return (hb @ w2[e]) * g[:, None].astype(x.dtype)

    y_buf = lax.map(run_block, (blk_expert, tok_buf.reshape(n_blocks, MOE_BLOCK), gate_buf.reshape(n_blocks, MOE_BLOCK)))
    y = jnp.zeros_like(xt).at[tok_buf].add(y_buf.reshape(-1, d))
    return y.reshape(b, s, d)


def setup_inputs(seed: int = 0) -> dict:
    key = jax.random.key(seed)
    ks = jax.random.split(key, 32)
    L = DEPTH

    def nrm(k, shape, scale):
        return jax.random.normal(k, shape, jnp.float32) * scale

    return {
        'x': nrm(ks[0], (BATCH, SEQ, D_MODEL), 1.0),
        'ln_in_g': 1.0 + nrm(ks[1], (D_MODEL,), 0.02),
        'ln_in_b': nrm(ks[2], (D_MODEL,), 0.02),
        't5_table': nrm(ks[3], (T5_BUCKETS, DIFF_HEADS), 0.5),
        'w_in': nrm(ks[4], (L, D_MODEL, D_IN), D_MODEL ** -0.5),
        'b_forget': 3.0 + nrm(ks[5], (L, FOX_HEADS), 0.1),
        'diff_lambda': nrm(ks[6], (L, 4, DIFF_DH), 0.1),
        'diff_norm_g': 1.0 + nrm(ks[7], (L, 2 * DIFF_DH), 0.02),
        'chunk_rel_bias': nrm(ks[8], (L, 2 * REL_CLIP + 1, CHUNK_HEADS), 0.5),
        'mla_q_norm_g': 1.0 + nrm(ks[9], (L, MLA_Q_LORA), 0.02),
        'mla_kv_norm_g': 1.0 + nrm(ks[10], (L, MLA_KV_LORA), 0.02),
        'mla_w_uq': nrm(ks[11], (L, MLA_Q_LORA, MLA_HEADS * (MLA_NOPE + MLA_ROPE)), MLA_Q_LORA ** -0.5),
        'mla_w_ukv': nrm(ks[12], (L, MLA_KV_LORA, MLA_HEADS * (MLA_NOPE + MLA_V)), MLA_KV_LORA ** -0.5),
        'w_gate': nrm(ks[13], (L, N_BRANCHES, D_MODEL, D_MODEL), D_MODEL ** -0.5),
        'b_gate': nrm(ks[14], (L, N_BRANCHES, D_MODEL), 0.02),
        'w_branch': nrm(ks[15], (L, N_BRANCHES, BRANCH_WIDTH, D_MODEL), BRANCH_WIDTH ** -0.5 * DEEPNORM_BETA),
        'w_out': nrm(ks[16], (L, D_MODEL, D_MODEL), D_MODEL ** -0.5 * DEEPNORM_BETA),
        'ln1_g': 1.0 + nrm(ks[17], (L, D_MODEL), 0.02),
        'ln1_b': nrm(ks[18], (L, D_MODEL), 0.02),
        'router_group_w': nrm(ks[19], (L, D_MODEL, N_GROUPS), D_MODEL ** -0.5),
        'router_group_b': nrm(ks[20], (L, N_GROUPS), 0.01),
        'router_expert_w': nrm(ks[21], (L, D_MODEL, N_EXPERTS), D_MODEL ** -0.5),
        'router_expert_b': nrm(ks[22], (L, N_EXPERTS), 0.01),
        'expert_w1': nrm(ks[23], (L, N_EXPERTS, D_MODEL, D_EXPERT), D_MODEL ** -0.5),
        'expert_w3': nrm(ks[24], (L, N_EXPERTS, D_MODEL, D_EXPERT), D_MODEL ** -0.5),
        'expert_w2': nrm(ks[25], (L, N_EXPERTS, D_EXPERT, D_MODEL), D_EXPERT ** -0.5 * DEEPNORM_BETA),
        'ln2_g': 1.0 + nrm(ks[26], (L, D_MODEL), 0.02),
        'ln2_b': nrm(ks[27], (L, D_MODEL), 0.02),
    }


def reference(x, ln_in_g, ln_in_b, t5_table, w_in, b_forget, diff_lambda, diff_norm_g, chunk_rel_bias,
              mla_q_norm_g, mla_kv_norm_g, mla_w_uq, mla_w_ukv, w_gate, b_gate, w_branch, w_out,
              ln1_g, ln1_b, router_group_w, router_group_b, router_expert_w, router_expert_b,
              expert_w1, expert_w3, expert_w2, ln2_g, ln2_b):
    s = x.shape[1]
    inv_freq = jnp.power(ROPE_BASE, -jnp.arange(MLA_ROPE // 2, dtype=jnp.float32) * 2.0 / MLA_ROPE)
    ang = jnp.arange(s, dtype=jnp.float32)[:, None] * inv_freq[None, :]
    cos, sin = jnp.cos(ang), jnp.sin(ang)
    h = layer_norm(x, ln_in_g, ln_in_b)
    for l in range(DEPTH):
        mix = mixing_sublayer(h, l, t5_table, cos, sin, w_in[l], b_forget[l], diff_lambda[l], diff_norm_g[l],
                              chunk_rel_bias[l], mla_q_norm_g[l], mla_kv_norm_g[l], mla_w_uq[l], mla_w_ukv[l],
                              w_gate[l], b_gate[l], w_branch[l], w_out[l])
        h = layer_norm(DEEPNORM_ALPHA * h + mix, ln1_g[l], ln1_b[l])
        ffn = hierarchical_moe(h, router_group_w[l], router_group_b[l], router_expert_w[l], router_expert_b[l],
                               expert_w1[l], expert_w3[l], expert_w2[l])
        h = layer_norm(DEEPNORM_ALPHA * h + ffn, ln2_g[l], ln2_b[l])
    return h
```

```python
import contextlib
import math
import numpy as np
import concourse.bass as bass
import concourse.mybir as mybir
from concourse.bass_utils import run_bass_kernel_spmd

F32 = mybir.dt.float32
BF16 = mybir.dt.bfloat16
AF = mybir.ActivationFunctionType
ALU = mybir.AluOpType
AX = mybir.AxisListType

D = 1024
KC = 8
DEPTH = 2
ALPHA = (2 * DEPTH) ** 0.25
EPS = 1e-5
NEXP = 32
DE = 512
C_DK, C_CK, C_FK, C_MKV, C_MKR, C_MKRS, C_FF = 0, 256, 512, 768, 896, 928, 960
C_Q = 964
C_DQ, C_CQ, C_FQ, C_MQ = C_Q, C_Q + 256, C_Q + 512, C_Q + 768
C_V = C_Q + 1024
WEXT = C_V + 768
MIX = ("diff", "chunk", "mla", "fox")
VCOL = {"diff": 0, "chunk": 256, "fox": 512, "mla": 768}


class Sched:
    ENG = ("pe", "act", "dve", "pool", "sp")
    NDQ = 6

    def __init__(self, nc, es):
        self.nc = nc
        self.eng = {"pe": nc.tensor, "act": nc.scalar, "dve": nc.vector, "pool": nc.gpsimd, "sp": nc.sync}
        self.es = es
        self.semobjs = {}
        self.nsem = 0
        self.ekey = {}
        self.cnt = {}
        for e in self.ENG:
            self._new_eng_sem(e)
        self.seen = {e: {} for e in self.ENG}
        self.dq = {}
        for q in ("sp", "act", "pool"):
            self.dq[q] = {"keys": [self._new_sem(("d", q)) for i in range(self.NDQ)],
                          "cnt": [0] * self.NDQ, "nxt": 0}
        self.lastw = {}
        self.readers = {}
        self.ninst = 0

    LIMIT = 30000

    def _new_sem(self, tag):
        self.nsem += 1
        key = (tag, self.nsem)
        self.semobjs[key] = self.es.enter_context(self.nc.semaphore(f"s{self.nsem}"))
        return key

    def _new_eng_sem(self, e):
        self.ekey[e] = self._new_sem(e)
        self.cnt[e] = 0

    def _semobj(self, key):
        return self.semobjs[key]

    def _wait(self, e, deps):
        need = {}
        for k, v in deps:
            if k[0] == "pe" and e == "pe":
                continue
            if v > need.get(k, 0):
                need[k] = v
        for k, v in need.items():
            if self.seen[e].get(k, 0) >= v:
                continue
            self.eng[e].wait_ge(self._semobj(k), v)
            self.seen[e][k] = v

    def _deps(self, reads, writes):
        deps = []
        for k in reads:
            t = self.lastw.get(k)
            if t:
                deps.append(t)
        for k in writes:
            t = self.lastw.get(k)
            if t:
                deps.append(t)
            deps.extend(self.readers.get(k, ()))
        return deps

    def _record(self, tok, reads, writes):
        for k in reads:
            self.readers.setdefault(k, []).append(tok)
        for k in writes:
            self.lastw[k] = tok
            self.readers[k] = []

    def op(self, e, fn, reads=(), writes=()):
        self._wait(e, self._deps(reads, writes))
        inst = fn(self.eng[e])
        if self.cnt[e] >= self.LIMIT:
            self._new_eng_sem(e)
        self.cnt[e] += 1
        inst.then_inc(self.semobjs[self.ekey[e]], 1)
        self._record((self.ekey[e], self.cnt[e]), reads, writes)
        self.ninst += 1

    def dma(self, q, out, in_, reads=(), writes=(), **kw):
        st = self.dq[q]
        i = st["nxt"]
        st["nxt"] = (i + 1) % self.NDQ
        deps = self._deps(reads, writes)
        if st["cnt"][i]:
            deps.append((st["keys"][i], st["cnt"][i]))
        self._wait(q, deps)
        if st["cnt"][i] >= self.LIMIT:
            st["keys"][i] = self._new_sem(("d", q))
            st["cnt"][i] = 0
        self.eng[q].dma_start(out=out, in_=in_, **kw).then_inc(self.semobjs[st["keys"][i]], 16)
        st["cnt"][i] += 16
        self._record((st["keys"][i], st["cnt"][i]), reads, writes)
        self.ninst += 1

    def _all(self):
        deps = []
        for q, st in self.dq.items():
            for i in range(self.NDQ):
                if st["cnt"][i]:
                    deps.append((st["keys"][i], st["cnt"][i]))
        for e in self.ENG:
            if self.cnt[e]:
                deps.append((self.ekey[e], self.cnt[e]))
        return deps

    def barrier(self):
        deps = self._all()
        for e in self.ENG:
            self._wait(e, [d for d in deps if d[0] != self.ekey[e]])

    def finish(self):
        self.barrier()


def build_program(S, layers, own_by_layer, n_in_layers=DEPTH):
    NB = S // 128
    NG = S // 512
    nc = bass.Bass("TRN2", target_bir_lowering=False)
    L = n_in_layers

    def din(name, shape, dt=F32):
        return nc.dram_tensor(name, list(shape), dt, kind="ExternalInput").ap()

    def dscr(name, shape, dt):
        return nc.dram_tensor(name, list(shape), dt, kind="Internal").ap()

    xin = din("xin", [S, D])
    lnin = din("lnin", [2, 128, D])
    w_ext = din("w_ext", [L, D, WEXT])
    nbf = din("nbf", [L, 4, 1])
    dlam = din("dlam", [L, 128, 128])
    dng = din("dng", [L, 64, 1])
    tbias = din("tbias", [4, 2, 128, 128])
    tconst = din("tconst", [128, 4])
    cbias = din("cbias", [L, 4, 2, 128, 128])
    cconst = din("cconst", [L, 128, 4])
    mqg = din("mqg", [L, 256, 1])
    mkvg = din("mkvg", [L, 128, 1])
    w_uq = din("w_uq", [L, 256, 768])
    w_ukv = din("w_ukv", [L, 128, 512])
    ropet = din("ropet", [2, 32, S])
    w_gate = din("w_gate", [L, 4, D, D])
    b_gate = din("b_gate", [L, 4, D])
    w_branch = din("w_branch", [L, 4, 256, D])
    w_out = din("w_out", [L, D, D])
    ln1 = din("ln1", [L, 2, 128, D])
    ln2 = din("ln2", [L, 2, 128, D])
    wr = din("wr", [L, D, 36])
    br = din("br", [L, 1, 36])
    ew1 = din("ew1", [L, NEXP, D, DE])
    ew3 = din("ew3", [L, NEXP, D, DE])
    ew2 = din("ew2", [L, NEXP, DE, D])
    keep = din("keep", [128, 1])
    ident = din("ident", [128, 128])
    trimask = din("trimask", [128, 128])
    NOmax = max(len(o) for o in own_by_layer.values()) * 128
    NOlast = len(own_by_layer[layers[-1]]) * 128
    out = nc.dram_tensor("out", [NOlast, D], F32, kind="ExternalOutput").ap()

    hres = dscr("hres", [S, D], F32)
    hmid = dscr("hmid", [S, D], F32)
    KT = {m: dscr("KT_" + m, [256, S], BF16) for m in ("diff", "chunk", "fox", "mla")}
    KTr = dscr("KT_mla_rope", [32, S], BF16)
    KC_f = dscr("KC_fox", [4, 3, S], BF16)
    QT = {m: dscr("QT_" + m, [384 if m == "mla" else 256, NOmax], BF16) for m in MIX}
    QC_f = dscr("QC_fox", [4, 3, NOmax], BF16)
    VV = dscr("VV", [16, 128, NB, 65], BF16)
    OT = dscr("OT", [4, 256, NOmax], BF16)
    h1d = dscr("h1d", [NOmax, D], F32)
    h1T = dscr("h1T", [D, NOmax], BF16)
    Gd = dscr("Gd", [NOmax, NEXP], F32)
    dscr_l = {"OTd": dscr("OTd", [2, 256, NOmax], F32)}

    es = contextlib.ExitStack()
    with es:
        sc = Sched(nc, es)

        def sb(name, shape, dt):
            return es.enter_context(nc.sbuf_tensor(name, list(shape), dt))

        PS = [es.enter_context(nc.psum_tensor(f"ps{i}", [128, 512], F32)) for i in range(8)]
        ps_rr = [0, 0]

        ps_pool = [list(range(7))]

        def psget(n=1):
            pool = ps_pool[0]
            if n == 2:
                pairs = [0, 2, 4]
                i = pairs[ps_rr[1] % 3]
                ps_rr[1] += 1
                return i
            i = pool[ps_rr[0] % len(pool)]
            ps_rr[0] += 1
            return i

        def pk(i, n=1):
            return [("ps", i + j) for j in range(n)]

        id_f = sb("id_f", [128, 128], F32)
        id_b = sb("id_b", [128, 128], BF16)
        tri_b = sb("tri_b", [128, 128], BF16)
        ones_f = sb("ones_f", [128, 512], F32)
        ones_b = sb("ones_b", [128, 128], BF16)
        eps_t = sb("eps_t", [128, 1], F32)
        sc.dma("sp", id_f[:], ident[:, :], writes=["id_f"])
        sc.dma("pool", id_b[:], ident[:, :], writes=["id_b"])
        sc.dma("pool", tri_b[:], trimask[:, :], writes=["tri_b"])
        sc.op("pool", lambda e: e.memset(ones_f[:], 1.0), writes=["ones_f"])
        sc.op("pool", lambda e: e.memset(ones_b[:], 1.0), writes=["ones_b"])
        sc.op("pool", lambda e: e.memset(eps_t[:], EPS), writes=["eps_t"])

        def layer_norm(src, skey, gb, gbkey, dst, dkey, tag, dst2=None, d2key=None):
            st = lnw["st"]; mv = lnw["mv"]; rs = lnw["rs"]; xn = lnw["xn"]
            for j in range(2):
                sc.op("dve", lambda e, j=j: e.bn_stats(st[:, j, :], src[:, j * 512:(j + 1) * 512]),
                      reads=[skey], writes=[("lnst", j)])
            sc.op("dve", lambda e: e.bn_aggr(mv[:], st[:].rearrange("p a b -> p (a b)")),
                  reads=[("lnst", 0), ("lnst", 1)], writes=["lnmv"])
            sc.op("act", lambda e: e.activation(rs[:], mv[:, 1:2], AF.Sqrt, bias=eps_t[:], scale=1.0),
                  reads=["lnmv", "eps_t"], writes=["lnrs"])
            sc.op("dve", lambda e: e.reciprocal(rs[:], rs[:]), reads=["lnrs"], writes=["lnrs"])
            sc.op("dve", lambda e: e.tensor_scalar(xn[:], src[:], mv[:, 0:1], rs[:], ALU.subtract, ALU.mult),
                  reads=[skey, "lnmv", "lnrs"], writes=["lnxn"])
            sc.op("pool", lambda e: e.tensor_tensor(xn[:], xn[:], gb[:, 0, :], ALU.mult),
                  reads=["lnxn", gbkey], writes=["lnxn"])
            sc.op("pool", lambda e: e.tensor_tensor(dst, xn[:], gb[:, 1, :], ALU.add),
                  reads=["lnxn", gbkey], writes=[dkey])
            if dst2 is not None:
                sc.op("act", lambda e: e.activation(dst2, dst, AF.Copy), reads=[dkey], writes=[d2key])

        lnw = {"st": sb("ln_st", [128, 2, 6], F32), "mv": sb("ln_mv", [128, 2], F32),
               "rs": sb("ln_rs", [128, 1], F32), "xn": sb("ln_xn", [128, D], F32)}

        for li, l in enumerate(layers):
            own = list(own_by_layer[l])
            NO = len(own) * 128
            ownidx = {b: i for i, b in enumerate(own)}
            src = xin if li == 0 else hmid
            last = (li == len(layers) - 1)
            dst = out if last else hmid
            lam_init = 0.8 - 0.6 * math.exp(-0.3 * l)

            with contextlib.ExitStack() as pa:
                def sba(name, shape, dt):
                    return pa.enter_context(nc.sbuf_tensor(f"A{l}_{name}", list(shape), dt))
                wext = sba("wext", [128, KC, WEXT], BF16)
                for kc in range(KC):
                    for c0_ in range(0, WEXT, 1024):
                        c1_ = min(WEXT, c0_ + 1024)
                        sc.dma("pool", wext[:, kc, c0_:c1_], w_ext[l, kc * 128:(kc + 1) * 128, c0_:c1_], writes=[("wext", kc)])
                wuq = sba("wuq", [128, 2, 768], BF16)
                for c in range(2):
                    sc.dma("pool", wuq[:, c, :], w_uq[l, c * 128:(c + 1) * 128, :], writes=["wuq"])
                wukv = sba("wukv", [128, 512], BF16)
                sc.dma("pool", wukv[:], w_ukv[l], writes=["wukv"])
                gq = sba("gq", [128, 2], F32)
                for c in range(2):
                    sc.dma("sp", gq[:, c:c + 1], mqg[l, c * 128:(c + 1) * 128, :], writes=["gq"])
                gkv = sba("gkv", [128, 1], F32)
                sc.dma("sp", gkv[:], mkvg[l], writes=["gkv"])
                nb4 = sba("nb4", [4, 1], F32)
                sc.dma("sp", nb4[:], nbf[l], writes=["nb4"])
                sc.op("dve", lambda e: e.tensor_scalar(nb4[:], nb4[:], -1.0, None, ALU.mult), reads=["nb4"], writes=["nb4"])
                if l == 0:
                    gbin = sba("gbin", [128, 2, D], F32)
                    sc.dma("sp", gbin[:], lnin.rearrange("a p d -> p a d"), writes=["gbin"])
                xb = [sba(f"xb{i}", [128, D], F32) for i in range(2)]
                hb = [sba(f"hb{i}", [128, D], F32) for i in range(2)]
                hbf = [sba(f"hbf{i}", [128, D], BF16) for i in range(2)]
                hT = [sba(f"hT{i}", [128, KC, 512], BF16) for i in range(2)]
                ev = [sba(f"ev{i}", [128, 512], BF16) for i in range(3)]
                evn = [0]
                vev = [sba(f"vev{i}", [128, 16, 4, 65], BF16) for i in range(2)]
                for i in range(2):
                    sc.op("pool", lambda e, i=i: e.memset(vev[i][:, :, :, 64:65], 1.0), writes=[("vev", i)])
                cqs = sba("cqs", [128, 2, 512], F32)
                ckvs = sba("ckvs", [128, 512], F32)
                sq = sba("sq", [128, 512], F32)
                rstd = sba("rstd", [128, 512], F32)
                cqn = sba("cqn", [128, 2, 512], BF16)
                ckvn = sba("ckvn", [128, 512], BF16)
                ropeA = sba("ropeA", [32, 2, 512], F32)
                ropeB = sba("ropeB", [96, 2, 512], F32)
                rt1 = sba("rt1", [96, 512], F32)
                rt2 = sba("rt2", [96, 512], F32)
                ffe = sba("ffe", [4, 512], F32)
                cum = [sba(f"cum{i}", [4, 512], F32) for i in range(2)]
                csp = sba("csp", [4, 3, 512], BF16)
                csn = sba("csn", [4, 3, 512], BF16)
                cr = sba("cr", [4, 512], F32)
                sc.op("pool", lambda e: e.memset(cum[1][:], 0.0), writes=[("cum", 1)])

                for g in range(NG):
                    hTg = hT[g % 2]
                    hk = ("hT", g % 2)
                    opos = [j for j in range(4) if (4 * g + j) in ownidx]
                    for j in range(4):
                        b = 4 * g + j
                        i2 = b % 2
                        sc.dma("sp", xb[i2][:], src[b * 128:(b + 1) * 128, :], reads=["src"], writes=[("xb", i2)])
                        if l == 0 and li == 0:
                            layer_norm(xb[i2], ("xb", i2), gbin, "gbin", hb[i2][:], ("hb", i2), "in",
                                       dst2=hbf[i2][:], d2key=("hbf", i2))
                            sc.dma("sp", hres[b * 128:(b + 1) * 128, :], hb[i2][:], reads=[("hb", i2)], writes=["hres"])
                        else:
                            sc.op("act", lambda e, i2=i2: e.activation(hbf[i2][:], xb[i2][:], AF.Copy),
                                  reads=[("xb", i2)], writes=[("hbf", i2)])
                        pt = 7
                        ptv = PS[pt][:].bitcast(BF16)
                        for kc in range(KC):
                            sc.op("pe", lambda e, kc=kc, i2=i2: e.transpose(ptv[:, kc * 128:(kc + 1) * 128],
                                                                           hbf[i2][:, kc * 128:(kc + 1) * 128], id_b[:]),
                                  reads=[("hbf", i2), "id_b"], writes=pk(pt))
                        sc.op("dve", lambda e, j=j: e.tensor_copy(hTg[:, :, j * 128:(j + 1) * 128],
                                                                  ptv.rearrange("p (k t) -> p k t", k=KC)),
                              reads=pk(pt), writes=[hk])

                    def fm(c0, n, cols=None, ncols=512):
                        p = psget()
                        rhs_of = (lambda kc: hTg[:, kc, :]) if cols is None else cols
                        for kc in range(KC):
                            sc.op("pe", lambda e, kc=kc: e.matmul(PS[p][0:n, 0:ncols], lhsT=wext[:, kc, c0:c0 + n],
                                                                   rhs=rhs_of(kc), start=(kc == 0), stop=(kc == KC - 1)),
                                  reads=[hk, ("wext", kc)], writes=pk(p))
                        return p

                    def evac_store(p, n, dram_ap, dkey, scale=None, ncols=512, eng="act"):
                        t = ev[evn[0] % 3]; tk = ("ev", evn[0] % 3); evn[0] += 1
                        if eng == "act":
                            sc.op("act", lambda e: e.activation(t[0:n, 0:ncols], PS[p][0:n, 0:ncols], AF.Copy,
                                                                scale=(1.0 if scale is None else scale)),
                                  reads=pk(p), writes=[tk])
                        else:
                            sc.op("dve", lambda e: e.tensor_copy(t[0:n, 0:ncols], PS[p][0:n, 0:ncols]),
                                  reads=pk(p), writes=[tk])
                        sc.dma("sp", dram_ap, t[0:n, 0:ncols], reads=[tk], writes=[dkey])

                    tsl = slice(g * 512, (g + 1) * 512)
                    for (mname, c0) in (("diff", C_DK), ("chunk", C_CK), ("fox", C_FK)):
                        for hh in range(2):
                            p = fm(c0 + hh * 128, 128)
                            evac_store(p, 128, KT[mname][hh * 128:(hh + 1) * 128, tsl], "KT_" + mname,
                                       eng=("act" if hh == 0 else "dve"))
                    if len(opos) == 4:
                        ocols = None; nq = 512
                    elif len(opos) == 2 and opos[1] - opos[0] == 2:
                        par = opos[0]
                        ocols = (lambda kc: hTg[:, kc, :].rearrange("p (b two t) -> p b two t", two=2, t=128)[:, :, par, :])
                        nq = 256
                    elif len(opos) == 0:
                        ocols = None; nq = 0
                    else:
                        raise NotImplementedError(opos)
                    if nq:
                        oq0 = ownidx[4 * g + opos[0]] * 128
                        qsl = slice(oq0, oq0 + nq)
                        for (mname, c0, dh) in (("diff", C_DQ, 32), ("chunk", C_CQ, 64), ("fox", C_FQ, 64)):
                            for hh in range(2):
                                p = fm(c0 + hh * 128, 128, cols=ocols, ncols=nq)
                                evac_store(p, 128, QT[mname][hh * 128:(hh + 1) * 128, qsl], "QT_" + mname,
                                           scale=dh ** -0.5, ncols=nq, eng="act")
                    p = fm(C_FF, 4)
                    cg = cum[g % 2]; cprev = cum[(g + 1) % 2]
                    sc.op("act", lambda e: e.activation(ffe[:], PS[p][0:4, :], AF.Exp, bias=nb4[:], scale=-1.0),
                          reads=pk(p) + ["nb4"], writes=["ffe"])
                    sc.op("act", lambda e: e.activation(ffe[:], ffe[:], AF.Ln, bias=1.0, scale=1.0),
                          reads=["ffe"], writes=["ffe"])
                    sc.op("dve", lambda e: e.tensor_tensor_scan(cg[:], ones_f[0:4, :], ffe[:], cprev[:, 511:512],
                                                                 ALU.mult, ALU.subtract),
                          reads=["ffe", "ones_f", ("cum", (g + 1) % 2)], writes=[("cum", g % 2)])
                    sc.op("dve", lambda e: e.tensor_copy(csp[:, 0, :], cg[:]), reads=[("cum", g % 2)], writes=["csp"])
                    sc.op("dve", lambda e: e.tensor_tensor(cr[:], cg[:], csp[:, 0, :], ALU.subtract),
                          reads=[("cum", g % 2), "csp"], writes=["cr"])
                    sc.op("dve", lambda e: e.tensor_copy(csp[:, 1, :], cr[:]), reads=["cr"], writes=["csp"])
                    sc.op("dve", lambda e: e.tensor_tensor(cr[:], cr[:], csp[:, 1, :], ALU.subtract),
                          reads=["cr", "csp"], writes=["cr"])
                    sc.op("dve", lambda e: e.tensor_copy(csp[:, 2, :], cr[:]), reads=["cr"], writes=["csp"])
                    sc.op("dve", lambda e: e.tensor_scalar(csn[:], csp[:], -1.0, None, ALU.mult), reads=["csp"], writes=["csn"])
                    sc.dma("sp", KC_f[:, :, tsl], csn[:], reads=["csn"], writes=["KC_f"])
                    for jj in opos:
                        o0 = ownidx[4 * g + jj] * 128
                        sc.dma("sp", QC_f[:, :, o0:o0 + 128], csp[:, :, jj * 128:(jj + 1) * 128], reads=["csp"], writes=["QC_f"])
                    sc.dma("sp", ropeA[:], ropet[:, :, tsl].rearrange("a r t -> r a t"), writes=["ropeA"])
                    sc.dma("sp", ropeB[64:96, :, :], ropet[:, :, tsl].rearrange("a r t -> r a t"), writes=["ropeB"])
                    p = fm(C_MKV, 128)
                    sc.op("act", lambda e: e.activation(ckvs[:], PS[p][:, :], AF.Copy), reads=pk(p), writes=["ckvs"])
                    sc.op("act", lambda e: e.activation(sq[:], PS[p][:, :], AF.Square), reads=pk(p), writes=["sq"])
                    p2 = psget()
                    sc.op("pe", lambda e: e.matmul(PS[p2][:, :], lhsT=ones_f[:, 0:128], rhs=sq[:], start=True, stop=True),
                          reads=["sq", "ones_f"], writes=pk(p2))
                    sc.op("act", lambda e: e.activation(rstd[:], PS[p2][:, :], AF.Sqrt, bias=eps_t[:], scale=1.0 / 128),
                          reads=pk(p2) + ["eps_t"], writes=["rstd"])
                    sc.op("dve", lambda e: e.reciprocal(rstd[:], rstd[:]), reads=["rstd"], writes=["rstd"])
                    sc.op("dve", lambda e: e.scalar_tensor_tensor(ckvn[:], ckvs[:], gkv[:, 0:1], rstd[:], ALU.mult, ALU.mult),
                          reads=["ckvs", "gkv", "rstd"], writes=["ckvn"])
                    for hh in range(2):
                        p = psget()
                        sc.op("pe", lambda e, hh=hh: e.matmul(PS[p][:, :], lhsT=wukv[:, hh * 128:(hh + 1) * 128], rhs=ckvn[:],
                                                               start=True, stop=True), reads=["wukv", "ckvn"], writes=pk(p))
                        evac_store(p, 128, KT["mla"][hh * 128:(hh + 1) * 128, tsl], "KT_mla", eng=("act" if hh else "dve"))
                    pA = fm(C_MKR, 32); pB = fm(C_MKRS, 32)
                    sc.op("dve", lambda e: e.tensor_tensor(rt1[0:32, :], PS[pA][0:32, :], ropeA[:, 0, :], ALU.mult),
                          reads=pk(pA) + ["ropeA"], writes=["rt1"])
                    sc.op("dve", lambda e: e.tensor_tensor(rt2[0:32, :], PS[pB][0:32, :], ropeA[:, 1, :], ALU.mult),
                          reads=pk(pB) + ["ropeA"], writes=["rt2"])
                    t = ev[evn[0] % 3]; tk = ("ev", evn[0] % 3); evn[0] += 1
                    sc.op("dve", lambda e: e.tensor_tensor(t[0:32, :], rt1[0:32, :], rt2[0:32, :], ALU.add),
                          reads=["rt1", "rt2"], writes=[tk])
                    sc.dma("sp", KTr[:, tsl], t[0:32, :], reads=[tk], writes=["KTr"])
                    if nq:
                        for c in range(2):
                            p = fm(C_MQ + c * 128, 128, cols=ocols, ncols=nq)
                            sc.op("act", lambda e, c=c: e.activation(cqs[:, c, 0:nq], PS[p][:, 0:nq], AF.Copy),
                                  reads=pk(p), writes=[("cqs", c)])
                        p2 = psget()
                        for c in range(2):
                            sc.op("act", lambda e, c=c: e.activation(sq[:, 0:nq], cqs[:, c, 0:nq], AF.Square),
                                  reads=[("cqs", c)], writes=["sq"])
                            sc.op("pe", lambda e, c=c: e.matmul(PS[p2][:, 0:nq], lhsT=ones_f[:, 0:128], rhs=sq[:, 0:nq],
                                                                 start=(c == 0), stop=(c == 1)),
                                  reads=["sq", "ones_f"], writes=pk(p2))
                        sc.op("act", lambda e: e.activation(rstd[:, 0:nq], PS[p2][:, 0:nq], AF.Sqrt, bias=eps_t[:], scale=1.0 / 256),
                              reads=pk(p2) + ["eps_t"], writes=["rstd"])
                        sc.op("dve", lambda e: e.reciprocal(rstd[:, 0:nq], rstd[:, 0:nq]), reads=["rstd"], writes=["rstd"])
                        for c in range(2):
                            sc.op("dve", lambda e, c=c: e.scalar_tensor_tensor(cqn[:, c, 0:nq], cqs[:, c, 0:nq], gq[:, c:c + 1],
                                                                                rstd[:, 0:nq], ALU.mult, ALU.mult),
                                  reads=[("cqs", c), "gq", "rstd"], writes=["cqn"])
                        qscale = 96 ** -0.5
                        for h in range(4):
                            pN = psget(); pS_ = psget()
                            for c in range(2):
                                sc.op("pe", lambda e, c=c, h=h: e.matmul(PS[pN][0:96, 0:nq], lhsT=wuq[:, c, h * 192:h * 192 + 96],
                                                                          rhs=cqn[:, c, 0:nq], start=(c == 0), stop=(c == 1)),
                                      reads=["wuq", "cqn"], writes=pk(pN))
                            for c in range(2):
                                sc.op("pe", lambda e, c=c, h=h: e.matmul(PS[pS_][0:96, 0:nq], lhsT=wuq[:, c, h * 192 + 96:h * 192 + 192],
                                                                          rhs=cqn[:, c, 0:nq], start=(c == 0), stop=(c == 1)),
                                      reads=["wuq", "cqn"], writes=pk(pS_))
                            t = ev[evn[0] % 3]; tk = ("ev", evn[0] % 3); evn[0] += 1
                            sc.op("act", lambda e, t=t: e.activation(t[0:64, 0:nq], PS[pN][0:64, 0:nq], AF.Copy, scale=qscale),
                                  reads=pk(pN), writes=[tk])
                            if ocols is None:
                                rc = ropeB[64:96, 0, :]; rs_ = ropeB[64:96, 1, :]
                            else:
                                rc = ropeB[64:96, 0, :].rearrange("p (b two t) -> p b two t", two=2, t=128)[:, :, par, :]
                                rs_ = ropeB[64:96, 1, :].rearrange("p (b two t) -> p b two t", two=2, t=128)[:, :, par, :]
                            def v3(ap):
                                return ap if ocols is None else ap.rearrange("p (b t) -> p b t", t=128)
                            sc.op("dve", lambda e: e.scalar_tensor_tensor(v3(rt1[64:96, 0:nq]), v3(PS[pN][64:96, 0:nq]), qscale, rc,
                                                                          ALU.mult, ALU.mult),
                                  reads=pk(pN) + ["ropeB"], writes=["rt1"])
                            sc.op("dve", lambda e: e.scalar_tensor_tensor(v3(rt2[64:96, 0:nq]), v3(PS[pS_][64:96, 0:nq]), qscale, rs_,
                                                                          ALU.mult, ALU.mult),
                                  reads=pk(pS_) + ["ropeB"], writes=["rt2"])
                            sc.op("dve", lambda e, t=t: e.tensor_tensor(t[64:96, 0:nq], rt1[64:96, 0:nq], rt2[64:96, 0:nq], ALU.add),
                                  reads=["rt1", "rt2", tk], writes=[tk])
                            sc.dma("sp", QT["mla"][h * 96:(h + 1) * 96, qsl], t[0:96, 0:nq], reads=[tk], writes=["QT_mla"])
                    vt = vev[g % 2]; vk = ("vev", g % 2)
                    for j in range(4):
                        pa_ = psget(); pb_ = psget()
                        for kc in range(KC):
                            sc.op("pe", lambda e, kc=kc: e.matmul(PS[pa_][:, :], lhsT=hTg[:, kc, j * 128:(j + 1) * 128],
                                                                   rhs=wext[:, kc, C_V:C_V + 512], start=(kc == 0), stop=(kc == KC - 1)),
                                  reads=[hk, ("wext", kc)], writes=pk(pa_))
                        for kc in range(KC):
                            sc.op("pe", lambda e, kc=kc: e.matmul(PS[pb_][:, 0:256], lhsT=hTg[:, kc, j * 128:(j + 1) * 128],
                                                                   rhs=wext[:, kc, C_V + 512:C_V + 768], start=(kc == 0), stop=(kc == KC - 1)),
                                  reads=[hk, ("wext", kc)], writes=pk(pb_))
                        sc.op("act", lambda e: e.activation(vt[:, 0:8, j, 0:64], PS[pa_][:, :].rearrange("p (a e) -> p a e", e=64), AF.Copy),
                              reads=pk(pa_), writes=[vk])
                        sc.op("dve", lambda e: e.tensor_copy(vt[:, 8:12, j, 0:64], PS[pb_][:, 0:256].rearrange("p (a e) -> p a e", e=64)),
                              reads=pk(pb_), writes=[vk])
                        pc_ = psget()
                        sc.op("pe", lambda e: e.matmul(PS[pc_][:, 0:256], lhsT=ckvn[:, j * 128:(j + 1) * 128], rhs=wukv[:, 256:512],
                                                       start=True, stop=True), reads=["ckvn", "wukv"], writes=pk(pc_))
                        sc.op("act", lambda e: e.activation(vt[:, 12:16, j, 0:64], PS[pc_][:, 0:256].rearrange("p (a e) -> p a e", e=64), AF.Copy),
                              reads=pk(pc_), writes=[vk])
                    sc.dma("sp", VV[:, :, 4 * g:4 * g + 4, :].rearrange("a p b e -> p a b e"), vt[:], reads=[vk], writes=["VV"])
                sc.barrier()
            if PHASES < 2:
                continue
            hsrc = hres if (l == 0 and li == 0) else src

            with contextlib.ExitStack() as pb:
                def sbb(name, shape, dt):
                    return pb.enter_context(nc.sbuf_tensor(f"B{l}_{name}", list(shape), dt))
                NOB = len(own)
                ps_pool[0] = [0, 1, 2, 3, 4]
                pon = [0]
                ktb = [sbb(f"kt{i}", [96, S], BF16) for i in range(2)]
                qtb = [sbb(f"qt{i}", [96, NO], BF16) for i in range(2)]
                vtb = [sbb(f"vt{i}", [128, NB, 65], BF16) for i in range(2)]
                ptb = [sbb(f"pt{i}", [128, 512], BF16) for i in range(3)]
                btile = sbb("btile", [128, 4, 2, 128], BF16)
                bstage = sbb("bstage", [128, 4, 2, 128], F32)
                bconst = sbb("bconst", [128, 4], F32)
                keept = sbb("keept", [128, 1], F32)
                sc.dma("sp", keept[:], keep[:, :], writes=["keept"])
                zero_c = sbb("zero_c", [128, 1], F32)
                sc.op("pool", lambda e: e.memset(zero_c[:], 0.0), writes=["zero_c"])
                osb = sbb("osb", [64, 512], F32)
                osb2 = sbb("osb2", [64, 512], F32)
                rden = sbb("rden", [1, 512], F32)
                rdb = sbb("rdb", [64, 512], F32)
                obf = [sbb(f"obf{i}", [64, 512], BF16) for i in range(2)]
                obf32 = [sbb(f"obf32_{i}", [64, 512], F32) for i in range(2)]
                ptn = [0]; otn = [0]
                osq = sbb("osq", [64, 512], F32)
                lamt = sbb("lamt", [128, 128], F32)
                lamv = sbb("lamv", [128, 4], F32)
                dgn = sbb("dgn", [64, 1], F32)
                sc.dma("sp", lamt[:], dlam[l], writes=["lamt"])
                sc.dma("sp", dgn[:], dng[l], writes=["dgn"])
                sc.op("dve", lambda e: e.tensor_tensor(lamt[:, 0:32], lamt[:, 0:32], lamt[:, 32:64], ALU.mult), reads=["lamt"], writes=["lamt"])
                sc.op("dve", lambda e: e.tensor_tensor(lamt[:, 64:96], lamt[:, 64:96], lamt[:, 96:128], ALU.mult), reads=["lamt"], writes=["lamt"])
                sc.op("dve", lambda e: e.tensor_reduce(lamv[:, 0:1], lamt[:, 0:32], AX.X, ALU.add), reads=["lamt"], writes=["lamv"])
                sc.op("dve", lambda e: e.tensor_reduce(lamv[:, 1:2], lamt[:, 64:96], AX.X, ALU.add), reads=["lamt"], writes=["lamv"])
                sc.op("act", lambda e: e.activation(lamv[:, 0:2], lamv[:, 0:2], AF.Exp), reads=["lamv"], writes=["lamv"])
                sc.op("dve", lambda e: e.tensor_tensor(lamv[:, 2:3], lamv[:, 0:1], lamv[:, 1:2], ALU.subtract), reads=["lamv"], writes=["lamv"])
                sc.op("dve", lambda e: e.tensor_scalar(lamv[:, 3:4], lamv[:, 2:3], lam_init, -1.0, ALU.add, ALU.mult), reads=["lamv"], writes=["lamv"])
                sc.op("dve", lambda e: e.tensor_scalar(dgn[:], dgn[:], 1.0 - lam_init, None, ALU.mult), reads=["dgn"], writes=["dgn"])

                def load_bias(dram_tiles, dram_const):
                    sc.dma("sp", bstage[:], dram_tiles.rearrange("h a k q -> k h a q"), writes=["bstage"])
                    sc.dma("sp", bconst[:], dram_const, writes=["bconst"])
                    for h in range(4):
                        sc.op("dve", lambda e, h=h: e.tensor_scalar(btile[:, h, :, :], bstage[:, h, :, :], bconst[:, h:h + 1], None, ALU.subtract),
                              reads=["bstage", "bconst"], writes=["btile"])

                OTd = dscr_l["OTd"]
                ldn = [0]
                for mi, m in enumerate(MIX):
                    if m not in DBG_MIX:
                        continue
                    if m == "diff":
                        load_bias(tbias, tconst)
                    elif m == "chunk":
                        load_bias(cbias[l], cconst[l])
                    dqk = {"diff": 32, "chunk": 64, "mla": 96, "fox": 70}[m]
                    QB = 1 if m == "chunk" else 4
                    nstream = 2 if m == "diff" else 1
                    tiles = [own[i:i + QB] for i in range(0, NOB, QB)]
                    for h in range(4):
                        vi = h % 2
                        sc.dma(BQ, vtb[vi][:], VV[VCOL[m] // 64 + h], reads=["VV"], writes=[("vt", vi)])
                        sc.op("dve", lambda e: e.tensor_scalar(vtb[vi][:, 0, :], vtb[vi][:, 0, :], keept[:, 0:1], None, ALU.mult),
                              reads=[("vt", vi), "keept"], writes=[("vt", vi)])
                        for s_ in range(nstream):
                            bi = ldn[0] % 2; ldn[0] += 1
                            kt = ktb[bi]; qt = qtb[bi]; kk = ("kt", bi); qk = ("qt", bi)
                            if m == "diff":
                                r0 = h * 64 + s_ * 32
                                sc.dma(BQ, kt[0:32, :], KT[m][r0:r0 + 32, :], reads=["KT_" + m], writes=[kk])
                                sc.dma(BQ, qt[0:32, :], QT[m][r0:r0 + 32, 0:NO], reads=["QT_" + m], writes=[qk])
                            elif m == "chunk":
                                sc.dma(BQ, kt[0:64, :], KT[m][h * 64:h * 64 + 64, :], reads=["KT_" + m], writes=[kk])
                                sc.dma(BQ, qt[0:64, :], QT[m][h * 64:h * 64 + 64, 0:NO], reads=["QT_" + m], writes=[qk])
                            elif m == "mla":
                                sc.dma(BQ, kt[0:64, :], KT[m][h * 64:h * 64 + 64, :], reads=["KT_" + m], writes=[kk])
                                sc.dma(BQ, kt[64:96, :], KTr[:, :], reads=["KTr"], writes=[kk])
                                sc.dma(BQ, qt[0:96, :], QT[m][h * 96:h * 96 + 96, 0:NO], reads=["QT_" + m], writes=[qk])
                            else:
                                sc.op("pool", lambda e, kt=kt: e.memset(kt[64:70, :], 1.0), writes=[kk])
                                sc.op("pool", lambda e, qt=qt: e.memset(qt[64:70, :], 1.0), writes=[qk])
                                sc.dma(BQ, kt[0:64, :], KT[m][h * 64:h * 64 + 64, :], reads=["KT_" + m], writes=[kk])
                                sc.dma(BQ, kt[67:70, :], KC_f[h, :, :], reads=["KC_f"], writes=[kk])
                                sc.dma(BQ, qt[0:64, :], QT[m][h * 64:h * 64 + 64, 0:NO], reads=["QT_" + m], writes=[qk])
                                sc.dma(BQ, qt[64:67, :], QC_f[h, :, 0:NO], reads=["QC_f"], writes=[qk])
                            for ti, blks in enumerate(tiles):
                                nqb = len(blks)
                                q0 = ti * QB * 128
                                lo = [max(0, bq - 4) if m == "chunk" else 0 for bq in blks]
                                kbs = list(range(min(lo), blks[-1] + 1))
                                po = 5 + (pon[0] % 2); pon[0] += 1
                                started = [False] * nqb

                                def near(kb, j):
                                    if m not in ("diff", "chunk"):
                                        return None
                                    if kb == blks[j]:
                                        return 0
                                    if kb == blks[j] - 1:
                                        return 1
                                    return None

                                def emit_qk(kb):
                                    js = [j for j in range(nqb) if lo[j] <= kb <= blks[j]]
                                    ja, jb = js[0], js[-1]
                                    p = psget()
                                    ksl = kt[0:dqk, kb * 128:(kb + 1) * 128]
                                    runs = []
                                    j = ja
                                    while j <= jb:
                                        kind = near(kb, j)
                                        if kind is not None:
                                            runs.append((j, j, kind)); j += 1
                                        else:
                                            j2 = j
                                            while j2 + 1 <= jb and near(kb, j2 + 1) is None:
                                                j2 += 1
                                            runs.append((j, j2, None)); j = j2 + 1
                                    for (a, b_, kind) in runs:
                                        cs = slice(a * 128, (b_ + 1) * 128)
                                        sc.op("pe", lambda e, cs=cs, kind=kind: e.matmul(PS[p][:, cs], lhsT=ksl, rhs=qt[0:dqk, q0 + cs.start:q0 + cs.stop],
                                                                                           start=True, stop=(kind is None)),
                                              reads=[kk, qk], writes=pk(p))
                                        if kind is not None:
                                            sc.op("pe", lambda e, cs=cs, kind=kind: e.matmul(PS[p][:, cs], lhsT=id_b[:], rhs=btile[:, h, kind, :],
                                                                                               start=False, stop=True),
                                                  reads=["id_b", "btile"], writes=pk(p))
                                    return (kb, p, ja, jb)

                                def emit_rest(item):
                                    kb, p, ja, jb = item
                                    cs = slice(ja * 128, (jb + 1) * 128)
                                    pi = ptn[0] % 3; ptn[0] += 1
                                    pt = ptb[pi]; ptk = ("pt", pi)
                                    bias_ap = bconst[:, h:h + 1] if m in ("diff", "chunk") else zero_c[:]
                                    sc.op("act", lambda e: e.activation(pt[:, cs], PS[p][:, cs], AF.Exp, bias=bias_ap, scale=1.0),
                                          reads=pk(p) + ["bconst", "zero_c"], writes=[ptk])
                                    for j in range(ja, jb + 1):
                                        c0 = j * 128
                                        if kb == blks[j]:
                                            if m == "fox":
                                                sc.op("pool", lambda e, c0=c0: e.tensor_tensor(pt[:, c0:c0 + 128], pt[:, c0:c0 + 128], tri_b[:], ALU.mult),
                                                      reads=[ptk, "tri_b"], writes=[ptk])
                                            else:
                                                sc.op("pool", lambda e, c0=c0: e.memset(pt[64:128, c0:c0 + 64], 0.0), writes=[ptk])
                                        if m == "chunk" and kb == blks[j] - 4:
                                            sc.op("pool", lambda e, c0=c0: e.memset(pt[0:64, c0 + 64:c0 + 128], 0.0), writes=[ptk])
                                    j = ja
                                    while j <= jb:
                                        j2 = j
                                        while j2 + 1 <= jb and started[j2 + 1] == started[j]:
                                            j2 += 1
                                        c2 = slice(j * 128, (j2 + 1) * 128)
                                        is_last = (kb == kbs[-1])
                                        sc.op("pe", lambda e, c2=c2, st=(not started[j]), is_last=is_last: e.matmul(
                                            PS[po][0:65, c2], lhsT=vtb[vi][:, kb, :], rhs=pt[:, c2], start=st, stop=is_last),
                                            reads=[("vt", vi), ptk], writes=pk(po))
                                        for jj in range(j, j2 + 1):
                                            started[jj] = True
                                        j = j2 + 1

                                pend = emit_qk(kbs[0])
                                for kb in kbs[1:]:
                                    nxt = emit_qk(kb)
                                    emit_rest(pend)
                                    pend = nxt
                                emit_rest(pend)
                                ncol = nqb * 128
                                sc.op("dve", lambda e: e.tensor_scalar(rden[0:1, 0:ncol], PS[po][64:65, 0:ncol], 1e-30, None, ALU.max), reads=pk(po), writes=["rden"])
                                sc.op("dve", lambda e: e.reciprocal(rden[0:1, 0:ncol], rden[0:1, 0:ncol]), reads=["rden"], writes=["rden"])
                                pbk = 7
                                sc.op("pe", lambda e: e.matmul(PS[pbk][0:64, 0:ncol], lhsT=ones_f[0:1, 0:64], rhs=rden[0:1, 0:ncol], start=True, stop=True),
                                      reads=["rden", "ones_f"], writes=pk(pbk))
                                sc.op("act", lambda e: e.activation(rdb[:, 0:ncol], PS[pbk][0:64, 0:ncol], AF.Copy), reads=pk(pbk), writes=["rdb"])
                                osl = slice(q0, q0 + ncol)
                                oi = otn[0] % 2; otn[0] += 1
                                if m != "diff":
                                    ob = obf[oi]; obk = ("obf", oi)
                                    tgt = OT[mi, h * 64:(h + 1) * 64, osl]; tk_ = "OT"
                                else:
                                    ob = obf32[oi]; obk = ("obf32", oi)
                                    tgt = OTd[s_, h * 64:(h + 1) * 64, osl]; tk_ = "OTd"
                                sc.op("dve", lambda e: e.tensor_tensor(ob[:, 0:ncol], PS[po][0:64, 0:ncol], rdb[:, 0:ncol], ALU.mult),
                                      reads=pk(po) + ["rdb"], writes=[obk])
                                sc.dma("sp", tgt, ob[:, 0:ncol], reads=[obk], writes=[tk_])
                    if m == "diff":
                        for h in range(4):
                            for c0 in range(0, NO, 512):
                                ncol = min(512, NO - c0)
                                sc.dma(BQ, osb[:, 0:ncol], OTd[0, h * 64:(h + 1) * 64, c0:c0 + ncol], reads=["OTd"], writes=["osb"])
                                sc.dma(BQ, osb2[:, 0:ncol], OTd[1, h * 64:(h + 1) * 64, c0:c0 + ncol], reads=["OTd"], writes=["osb2"])
                                sc.op("dve", lambda e: e.scalar_tensor_tensor(osb[:, 0:ncol], osb2[:, 0:ncol], lamv[0:64, 3:4], osb[:, 0:ncol], ALU.mult, ALU.add),
                                      reads=["osb", "osb2", "lamv"], writes=["osb"])
                                sc.op("act", lambda e: e.activation(osq[:, 0:ncol], osb[:, 0:ncol], AF.Square), reads=["osb"], writes=["osq"])
                                pq = 7
                                sc.op("pe", lambda e: e.matmul(PS[pq][0:64, 0:ncol], lhsT=ones_f[0:64, 0:64], rhs=osq[:, 0:ncol], start=True, stop=True),
                                      reads=["osq", "ones_f"], writes=pk(pq))
                                sc.op("act", lambda e: e.activation(rdb[:, 0:ncol], PS[pq][0:64, 0:ncol], AF.Sqrt, bias=eps_t[0:64, :], scale=1.0 / 64),
                                      reads=pk(pq) + ["eps_t"], writes=["rdb"])
                                sc.op("dve", lambda e: e.reciprocal(rdb[:, 0:ncol], rdb[:, 0:ncol]), reads=["rdb"], writes=["rdb"])
                                oi = otn[0] % 2; otn[0] += 1
                                ob = obf[oi]; obk = ("obf", oi)
                                sc.op("dve", lambda e: e.scalar_tensor_tensor(ob[:, 0:ncol], osb[:, 0:ncol], dgn[:, 0:1], rdb[:, 0:ncol], ALU.mult, ALU.mult),
                                      reads=["osb", "dgn", "rdb"], writes=[obk])
                                sc.dma("sp", OT[0, h * 64:(h + 1) * 64, c0:c0 + ncol], ob[:, 0:ncol], reads=[obk], writes=["OT"])
                ps_pool[0] = list(range(7))
                sc.barrier()
            if PHASES < 3:
                continue
            with contextlib.ExitStack() as pcx:
                def sbc(name, shape, dt):
                    return pcx.enter_context(nc.sbuf_tensor(f"C{l}_{name}", list(shape), dt))
                wg = sbc("wg", [128, 4, KC, D], BF16)
                for n in range(4):
                    for kc in range(KC):
                        sc.dma("pool", wg[:, n, kc, :], w_gate[l, n, kc * 128:(kc + 1) * 128, :], writes=["wg"])
                bg = sbc("bg", [1, 4, D], BF16)
                bgf = sbc("bgf", [1, D], F32)
                for n in range(4):
                    sc.dma("sp", bgf[:], b_gate[l, n:n + 1, :], writes=["bgf"])
                    sc.op("dve", lambda e, n=n: e.tensor_copy(bg[0:1, n, :], bgf[:]), reads=["bgf"], writes=["bg"])
                wb = sbc("wb", [128, 4, 2, D], BF16)
                for n in range(4):
                    for c in range(2):
                        sc.dma("pool", wb[:, n, c, :], w_branch[l, n, c * 128:(c + 1) * 128, :], writes=["wb"])
                wo = sbc("wo", [128, KC, D], BF16)
                for kc in range(KC):
                    sc.dma("pool", wo[:, kc, :], w_out[l, kc * 128:(kc + 1) * 128, :], writes=["wo"])
                g1 = sbc("g1", [128, 2, D], F32)
                sc.dma("sp", g1[:], ln1[l].rearrange("a p d -> p a d"), writes=["g1"])
                wrt = sbc("wrt", [128, KC, 36], F32)
                sc.dma("sp", wrt[:], wr[l].rearrange("(k p) n -> p k n", p=128), writes=["wrt"])
                brt = sbc("brt", [1, 36], F32)
                sc.dma("sp", brt[:], br[l], writes=["brt"])
                hblk = [sbc(f"hblk{i}", [128, D], F32) for i in range(2)]
                hbfc = sbc("hbfc", [128, D], BF16)
                hTc = sbc("hTc", [128, KC, 128], BF16)
                ott = [sbc(f"ott{i}", [128, 4, 2, 512], BF16) for i in range(2)]
                merged = sbc("merged", [128, D], F32)
                gsb = [sbc(f"gsb{i}", [128, 512], F32) for i in range(2)]
                gtmp = [sbc(f"gtmp{i}", [128, 512], F32) for i in range(2)]
                mbf = sbc("mbf", [128, D], BF16)
                mT = sbc("mT", [128, KC, 128], BF16)
                rr = sbc("rr", [128, D], F32)
                h1s = [sbc(f"h1s{i}", [128, D], F32) for i in range(2)]
                h1hi = sbc("h1hi", [128, D], BF16)
                h1lo = sbc("h1lo", [128, D], BF16)
                h1Tl = sbc("h1Tl", [128, KC, 128], BF16)
                wrh = sbc("wrh", [128, KC, 36], BF16)
                wrl = sbc("wrl", [128, KC, 36], BF16)
                brh = sbc("brh", [1, 36], BF16)
                brl = sbc("brl", [1, 36], BF16)
                sc.op("dve", lambda e: e.tensor_copy(wrh[:], wrt[:]), reads=["wrt"], writes=["wrhl"])
                sc.op("dve", lambda e: e.tensor_tensor(wrl[:], wrt[:], wrh[:], ALU.subtract), reads=["wrt", "wrhl"], writes=["wrhl"])
                sc.op("dve", lambda e: e.tensor_copy(brh[:], brt[:]), reads=["brt"], writes=["wrhl"])
                sc.op("dve", lambda e: e.tensor_tensor(brl[:], brt[:], brh[:], ALU.subtract), reads=["brt", "wrhl"], writes=["wrhl"])
                h1Tb = [sbc(f"h1Tb{i}", [128, KC, 512], BF16) for i in range(2)]
                lgs = sbc("lgs", [128, 36], F32)
                rw = sbc("rw", [128, 16], F32)
                ohg = sbc("ohg", [128, 4], F32)
                sel = sbc("sel", [128, 8], F32)
                m8 = sbc("m8", [128, 8], F32)
                mk1 = sbc("mk1", [128, 8], F32)
                mk2 = sbc("mk2", [128, 8], F32)
                Gs = [sbc(f"Gs{i}", [128, NEXP], F32) for i in range(2)]
                gn = [0]
                for oi, b in enumerate(own):
                    i2 = oi % 2
                    hbk = ("hblk", i2)
                    sc.dma("sp", hblk[i2][:], hsrc[b * 128:(b + 1) * 128, :], reads=["hres", "src"], writes=[hbk])
                    og = (oi // 4) % 2
                    if oi % 4 == 0:
                        nt4 = min(512, NO - oi * 128)
                        for m_ in range(4):
                            sc.dma("act", ott[og][:, m_, :, 0:nt4], OT[m_, :, oi * 128:oi * 128 + nt4].rearrange("(c p) t -> p c t", p=128),
                                   reads=["OT"], writes=[("ott", og)])
                    osl_ = slice((oi % 4) * 128, (oi % 4) * 128 + 128)
                    sc.op("act", lambda e: e.activation(hbfc[:], hblk[i2][:], AF.Copy), reads=[hbk], writes=["hbfc"])
                    ptv = PS[7][:].bitcast(BF16)
                    for kc in range(KC):
                        sc.op("pe", lambda e, kc=kc: e.transpose(ptv[:, kc * 128:(kc + 1) * 128], hbfc[:, kc * 128:(kc + 1) * 128], id_b[:]),
                              reads=["hbfc", "id_b"], writes=pk(7))
                    sc.op("dve", lambda e: e.tensor_copy(hTc[:], ptv.rearrange("p (k t) -> p k t", k=KC)), reads=pk(7), writes=["hTc"])
                    for n in range(4):
                        for half in range(2):
                            hs = slice(half * 512, (half + 1) * 512)
                            pg = psget()
                            for kc in range(KC):
                                sc.op("pe", lambda e, kc=kc: e.matmul(PS[pg][:, :], lhsT=hTc[:, kc, :], rhs=wg[:, n, kc, hs], start=(kc == 0), stop=False),
                                      reads=["hTc", "wg"], writes=pk(pg))
                            sc.op("pe", lambda e: e.matmul(PS[pg][:, :], lhsT=ones_b[0:1, 0:128], rhs=bg[0:1, n, hs], start=False, stop=True),
                                  reads=["ones_b", "bg"], writes=pk(pg))
                            gi = gn[0] % 2; gn[0] += 1
                            sc.op("act", lambda e: e.activation(gsb[gi][:], PS[pg][:, :], AF.Sigmoid), reads=pk(pg), writes=[("gsb", gi)])
                            pp = psget()
                            for c in range(2):
                                sc.op("pe", lambda e, c=c: e.matmul(PS[pp][:, :], lhsT=ott[og][:, n, c, osl_], rhs=wb[:, n, c, hs], start=(c == 0), stop=(c == 1)),
                                      reads=[("ott", og), "wb"], writes=pk(pp))
                            if n == 0:
                                sc.op("dve", lambda e: e.tensor_tensor(merged[:, hs], gsb[gi][:], PS[pp][:, :], ALU.mult),
                                      reads=[("gsb", gi)] + pk(pp), writes=[("merged", half)])
                            else:
                                sc.op("dve", lambda e: e.tensor_tensor(gtmp[gi][:], gsb[gi][:], PS[pp][:, :], ALU.mult),
                                      reads=[("gsb", gi)] + pk(pp), writes=[("gtmp", gi)])
                                sc.op("pool", lambda e: e.tensor_tensor(merged[:, hs], merged[:, hs], gtmp[gi][:], ALU.add),
                                      reads=[("gtmp", gi), ("merged", half)], writes=[("merged", half)])
                    if CLVL < 2:
                        continue
                    sc.op("act", lambda e: e.activation(mbf[:], merged[:], AF.Copy), reads=[("merged", 0), ("merged", 1)], writes=["mbf"])
                    for kc in range(KC):
                        sc.op("pe", lambda e, kc=kc: e.transpose(ptv[:, kc * 128:(kc + 1) * 128], mbf[:, kc * 128:(kc + 1) * 128], id_b[:]),
                              reads=["mbf", "id_b"], writes=pk(7))
                    sc.op("dve", lambda e: e.tensor_copy(mT[:], ptv.rearrange("p (k t) -> p k t", k=KC)), reads=pk(7), writes=["mT"])
                    if CLVL < 2.1:
                        continue
                    pm = psget(2)
                    for half in range(2):
                        hs = slice(half * 512, (half + 1) * 512)
                        for kc in range(KC):
                            sc.op("pe", lambda e, kc=kc: e.matmul(PS[pm + half][:, :], lhsT=mT[:, kc, :], rhs=wo[:, kc, hs], start=(kc == 0), stop=(kc == KC - 1)),
                                  reads=["mT", "wo"], writes=pk(pm + half))
                        sc.op("dve", lambda e: e.scalar_tensor_tensor(rr[:, hs], hblk[i2][:, hs], ALPHA, PS[pm + half][:, :], ALU.mult, ALU.add),
                              reads=[hbk] + pk(pm + half), writes=["rr"])
                    if CLVL < 2.2:
                        continue
                    layer_norm(rr, "rr", g1, "g1", h1s[i2][:], ("h1s", i2), "ln1")
                    if CLVL < 2.3:
                        continue
                    sc.dma("sp", h1d[oi * 128:(oi + 1) * 128, :], h1s[i2][:], reads=[("h1s", i2)], writes=["h1d"])
                    if CLVL < 3:
                        continue
                    sc.op("act", lambda e: e.activation(h1hi[:], h1s[i2][:], AF.Copy), reads=[("h1s", i2)], writes=["h1hi"])
                    sc.op("dve", lambda e: e.tensor_tensor(h1lo[:], h1s[i2][:], h1hi[:], ALU.subtract), reads=[("h1s", i2), "h1hi"], writes=["h1lo"])
                    for kc in range(KC):
                        sc.op("pe", lambda e, kc=kc: e.transpose(ptv[:, kc * 128:(kc + 1) * 128], h1hi[:, kc * 128:(kc + 1) * 128], id_b[:]),
                              reads=["h1hi", "id_b"], writes=pk(7))
                    sc.op("dve", lambda e: e.tensor_copy(h1Tb[og][:, :, osl_], ptv.rearrange("p (k t) -> p k t", k=KC)), reads=pk(7), writes=[("h1Tb", og)])
                    for kc in range(KC):
                        sc.op("pe", lambda e, kc=kc: e.transpose(ptv[:, kc * 128:(kc + 1) * 128], h1lo[:, kc * 128:(kc + 1) * 128], id_b[:]),
                              reads=["h1lo", "id_b"], writes=pk(7))
                    sc.op("dve", lambda e: e.tensor_copy(h1Tl[:], ptv.rearrange("p (k t) -> p k t", k=KC)), reads=pk(7), writes=["h1Tl"])
                    if oi % 4 == 3 or oi == len(own) - 1:
                        o0_ = (oi // 4) * 512
                        n_ = (oi % 4 + 1) * 128
                        sc.dma("sp", h1T[:, o0_:o0_ + n_].rearrange("(k p) t -> p k t", p=128), h1Tb[og][:, :, 0:n_], reads=[("h1Tb", og)], writes=["h1T"])
                    if CLVL < 4:
                        continue
                    pr = psget()
                    first = True
                    for kc in range(KC):
                        for (lt, ltk, rt_) in ((h1Tb[og][:, kc, osl_], ("h1Tb", og), wrh), (h1Tb[og][:, kc, osl_], ("h1Tb", og), wrl), (h1Tl[:, kc, :], "h1Tl", wrh)):
                            sc.op("pe", lambda e, lt=lt, rt_=rt_, kc=kc, first=first: e.matmul(PS[pr][:, 0:36], lhsT=lt, rhs=rt_[:, kc, :], start=first, stop=False),
                                  reads=[ltk, "wrhl"], writes=pk(pr))
                            first = False
                    sc.op("pe", lambda e: e.matmul(PS[pr][:, 0:36], lhsT=ones_b[0:1, 0:128], rhs=brh[0:1, :], start=False, stop=False),
                          reads=["ones_b", "wrhl"], writes=pk(pr))
                    sc.op("pe", lambda e: e.matmul(PS[pr][:, 0:36], lhsT=ones_b[0:1, 0:128], rhs=brl[0:1, :], start=False, stop=True),
                          reads=["ones_b", "wrhl"], writes=pk(pr))
                    sc.op("act", lambda e: e.activation(lgs[:], PS[pr][:, 0:36], AF.Copy), reads=pk(pr), writes=["lgs"])
                    if CLVL < 5:
                        continue
                    V = sc.op
                    V("dve", lambda e: e.tensor_reduce(rw[:, 0:1], lgs[:, 0:4], AX.X, ALU.max), reads=["lgs"], writes=["rw0"])
                    V("dve", lambda e: e.tensor_scalar(rw[:, 1:2], rw[:, 0:1], -1.0, None, ALU.mult), reads=["rw0"], writes=["rw1"])
                    V("act", lambda e: e.activation(ohg[:], lgs[:, 0:4], AF.Exp, bias=rw[:, 1:2], scale=1.0, accum_out=rw[:, 2:3]),
                      reads=["lgs", "rw1"], writes=["ohg", "rw2"])
                    V("dve", lambda e: e.reciprocal(rw[:, 3:4], rw[:, 2:3]), reads=["rw2"], writes=["rw3"])
                    V("dve", lambda e: e.tensor_scalar(ohg[:], lgs[:, 0:4], rw[:, 0:1], None, ALU.is_equal), reads=["lgs", "rw0", "ohg"], writes=["ohg"])
                    V("dve", lambda e: e.tensor_scalar(sel[:], lgs[:, 4:12], ohg[:, 0:1], None, ALU.mult), reads=["lgs", "ohg"], writes=["sel"])
                    for g_ in range(1, 4):
                        V("dve", lambda e, g_=g_: e.scalar_tensor_tensor(sel[:], lgs[:, 4 + 8 * g_:12 + 8 * g_], ohg[:, g_:g_ + 1], sel[:], ALU.mult, ALU.add),
                          reads=["lgs", "ohg", "sel"], writes=["sel"])
                    V("dve", lambda e: e.max(m8[:], sel[:]), reads=["sel"], writes=["m8"])
                    V("dve", lambda e: e.tensor_tensor(rw[:, 4:5], m8[:, 1:2], m8[:, 0:1], ALU.subtract), reads=["m8"], writes=["rw4"])
                    V("act", lambda e: e.activation(rw[:, 5:6], rw[:, 4:5], AF.Exp), reads=["rw4"], writes=["rw5"])
                    V("dve", lambda e: e.tensor_scalar(rw[:, 6:7], rw[:, 5:6], 1.0, None, ALU.add), reads=["rw5"], writes=["rw6"])
                    V("dve", lambda e: e.reciprocal(rw[:, 6:7], rw[:, 6:7]), reads=["rw6"], writes=["rw6"])
                    V("dve", lambda e: e.tensor_tensor(rw[:, 7:8], rw[:, 6:7], rw[:, 3:4], ALU.mult), reads=["rw6", "rw3"], writes=["rw7"])
                    V("dve", lambda e: e.tensor_tensor(rw[:, 8:9], rw[:, 7:8], rw[:, 5:6], ALU.mult), reads=["rw7", "rw5"], writes=["rw8"])
                    V("dve", lambda e: e.tensor_scalar(mk1[:], sel[:], m8[:, 0:1], rw[:, 7:8], ALU.is_equal, ALU.mult), reads=["sel", "m8", "rw7"], writes=["mk1"])
                    V("dve", lambda e: e.tensor_scalar(mk2[:], sel[:], m8[:, 1:2], rw[:, 8:9], ALU.is_equal, ALU.mult), reads=["sel", "m8", "rw8"], writes=["mk2"])
                    V("dve", lambda e: e.tensor_tensor(mk1[:], mk1[:], mk2[:], ALU.add), reads=["mk1", "mk2"], writes=["mk1"])
                    Gt_ = Gs[i2]
                    for g_ in range(4):
                        V("dve", lambda e, g_=g_: e.tensor_scalar(Gt_[:, 8 * g_:8 * g_ + 8], mk1[:], ohg[:, g_:g_ + 1], None, ALU.mult),
                          reads=["mk1", "ohg"], writes=[("Gs", i2)])
                    sc.dma("sp", Gd[oi * 128:(oi + 1) * 128, :], Gt_[:], reads=[("Gs", i2)], writes=["Gd"])
                if PHASES == 3:
                    sc.dma("sp", rr[:], h1d[0:128, :], reads=["h1d"], writes=["rr"])
                    sc.dma("sp", h1Tb[0][:, :, 0:128], h1T[:, 0:128].rearrange("(k p) t -> p k t", p=128), reads=["h1T"], writes=[("h1Tb", 0)])
                    sc.dma("sp", Gs[0][:], Gd[0:128, :], reads=["Gd"], writes=[("Gs", 0)])
                sc.barrier()
            if PHASES < 4:
                continue
            with contextlib.ExitStack() as pdx:
                def sbd(name, shape, dt):
                    return pdx.enter_context(nc.sbuf_tensor(f"D{l}_{name}", list(shape), dt))
                SB_ = min(16, len(own))
                Y = sbd("Y", [128, SB_, D], F32)
                xT = sbd("xT", [128, KC, SB_ * 128], BF16)
                Gt = sbd("Gt", [128, SB_, NEXP], F32)
                w1t = [sbd(f"w1t{i}", [128, KC, DE], BF16) for i in range(2)]
                w3t = [sbd(f"w3t{i}", [128, KC, DE], BF16) for i in range(2)]
                w2t = [sbd(f"w2t{i}", [128, 4, D], BF16) for i in range(2)]
                sil = [sbd(f"sil{i}", [128, 512], F32) for i in range(2)]
                hTe = [sbd(f"hTe{i}", [128, 4, 512], BF16) for i in range(2)]
                rr2 = sbd("rr2", [128, D], F32)
                g2 = sbd("g2", [128, 2, D], F32)
                sc.dma("sp", g2[:], ln2[l].rearrange("a p d -> p a d"), writes=["g2"])
                hh = [sbd(f"hh{i}", [128, D], F32) for i in range(2)]
                oo = [sbd(f"oo{i}", [128, D], F32) for i in range(2)]
                sn = [0]; hn = [0]
                for s0 in range(0, len(own), SB_):
                    nbs = min(SB_, len(own) - s0)
                    ntok = nbs * 128
                    sc.dma("sp", xT[:, :, 0:ntok], h1T[:, s0 * 128:s0 * 128 + ntok].rearrange("(k p) t -> p k t", p=128), reads=["h1T"], writes=["xT"])
                    sc.dma("sp", Gt[:, 0:nbs, :], Gd[s0 * 128:s0 * 128 + ntok, :].rearrange("(b p) e -> p b e", p=128), reads=["Gd"], writes=["Gt"])
                    for ex in range(NEXP):
                        wi = ex % 2
                        sc.dma("pool", w1t[wi][:], ew1[l, ex].rearrange("(k p) f -> p k f", p=128), writes=[("w1t", wi)])
                        sc.dma("pool", w3t[wi][:], ew3[l, ex].rearrange("(k p) f -> p k f", p=128), writes=[("w3t", wi)])
                        sc.dma("pool", w2t[wi][:], ew2[l, ex].rearrange("(c p) d -> p c d", p=128), writes=[("w2t", wi)])
                        for t0 in range(0, nbs, 4):
                            nbt = min(4, nbs - t0)
                            nt = nbt * 128
                            tcs = slice(t0 * 128, t0 * 128 + nt)
                            hi = hn[0] % 2; hn[0] += 1
                            for c in range(4):
                                fs = slice(c * 128, (c + 1) * 128)
                                pa_ = psget(); pb_ = psget()
                                for kc in range(KC):
                                    sc.op("pe", lambda e, kc=kc: e.matmul(PS[pa_][:, 0:nt], lhsT=w1t[wi][:, kc, fs], rhs=xT[:, kc, tcs], start=(kc == 0), stop=(kc == KC - 1)),
                                          reads=[("w1t", wi), "xT"], writes=pk(pa_))
                                for kc in range(KC):
                                    sc.op("pe", lambda e, kc=kc: e.matmul(PS[pb_][:, 0:nt], lhsT=w3t[wi][:, kc, fs], rhs=xT[:, kc, tcs], start=(kc == 0), stop=(kc == KC - 1)),
                                          reads=[("w3t", wi), "xT"], writes=pk(pb_))
                                si = sn[0] % 2; sn[0] += 1
                                sc.op("act", lambda e: e.activation(sil[si][:, 0:nt], PS[pa_][:, 0:nt], AF.Silu), reads=pk(pa_), writes=[("sil", si)])
                                sc.op("dve", lambda e: e.tensor_tensor(hTe[hi][:, c, 0:nt], sil[si][:, 0:nt], PS[pb_][:, 0:nt], ALU.mult),
                                      reads=[("sil", si)] + pk(pb_), writes=[("hTe", hi)])
                            for j in range(nbt):
                                for half in range(2):
                                    hs = slice(half * 512, (half + 1) * 512)
                                    py = psget()
                                    for c in range(4):
                                        sc.op("pe", lambda e, c=c: e.matmul(PS[py][:, :], lhsT=hTe[hi][:, c, j * 128:(j + 1) * 128], rhs=w2t[wi][:, c, hs],
                                                                             start=(c == 0), stop=(c == 3)),
                                              reads=[("hTe", hi), ("w2t", wi)], writes=pk(py))
                                    yk = ("Y", t0 + j, half)
                                    if ex == 0:
                                        sc.op("dve", lambda e: e.tensor_scalar(Y[:, t0 + j, hs], PS[py][:, :], Gt[:, t0 + j, ex:ex + 1], None, ALU.mult),
                                              reads=pk(py) + ["Gt"], writes=[yk])
                                    else:
                                        sc.op("dve", lambda e: e.scalar_tensor_tensor(Y[:, t0 + j, hs], PS[py][:, :], Gt[:, t0 + j, ex:ex + 1], Y[:, t0 + j, hs],
                                                                                      ALU.mult, ALU.add),
                                              reads=pk(py) + ["Gt", yk], writes=[yk])
                    for j in range(nbs):
                        oi = s0 + j
                        i2 = oi % 2
                        sc.dma("sp", hh[i2][:], h1d[oi * 128:(oi + 1) * 128, :], reads=["h1d"], writes=[("hh", i2)])
                        sc.op("dve", lambda e: e.scalar_tensor_tensor(rr2[:], hh[i2][:], ALPHA, Y[:, j, :], ALU.mult, ALU.add),
                              reads=[("hh", i2), ("Y", j, 0), ("Y", j, 1)], writes=["rr2"])
                        layer_norm(rr2, "rr2", g2, "g2", oo[i2][:], ("oo", i2), "ln2")
                        sc.dma("sp", dst[oi * 128:(oi + 1) * 128, :], oo[i2][:], reads=[("oo", i2)], writes=["dst" if last else "src"])
                sc.barrier()
        sc.finish()
    return nc, sc


PHASES = 4
DBG_MIX = MIX
CLVL = 9
BQ = 'act'


def _t5_bucket_np(rel):
    import jax, jax.numpy as jnp
    cpu = jax.devices("cpu")[0]
    with jax.default_device(cpu):
        rel = jnp.asarray(rel, dtype=jnp.int32)
        nb = 16
        max_exact = 8
        ret = jnp.where(rel > 0, nb, 0)
        n = jnp.abs(rel)
        nf = jnp.maximum(n, 1).astype(jnp.float32)
        large = max_exact + (jnp.log(nf / max_exact) / math.log(128 / max_exact) * (nb - max_exact)).astype(jnp.int32)
        large = jnp.minimum(large, nb - 1)
        return np.asarray(ret + jnp.where(n < max_exact, n, large))


def prep_weights(inp, S):
    f = lambda a: np.ascontiguousarray(np.asarray(a, dtype=np.float32))
    L = DEPTH
    w_in = f(inp["w_in"])
    r = np.arange
    cols = np.concatenate([r(256, 512), r(1024, 1280), r(2208, 2464), r(1792, 1920), r(1920, 1952),
                           r(1936, 1952), r(1920, 1936), r(2720, 2724),
                           r(0, 256), r(768, 1024), r(1952, 2208), r(1536, 1792),
                           r(512, 768), r(1280, 1536), r(2464, 2720)])
    assert len(cols) == WEXT
    d = {}
    d["w_ext"] = f(w_in[:, :, cols])
    d["lnin"] = f(np.stack([np.broadcast_to(inp["ln_in_g"], (128, D)), np.broadcast_to(inp["ln_in_b"], (128, D))]))
    d["nbf"] = f(np.asarray(inp["b_forget"]).reshape(L, 4, 1))
    d["dlam"] = f(np.broadcast_to(np.asarray(inp["diff_lambda"]).reshape(L, 1, 128), (L, 128, 128)))
    d["dng"] = f(np.asarray(inp["diff_norm_g"]).reshape(L, 64, 1))
    kq = r(128)
    rel0 = kq[:, None] - kq[None, :]
    rel1 = rel0 - 128
    t5 = f(inp["t5_table"])
    b0 = _t5_bucket_np(rel0); b1 = _t5_bucket_np(rel1)
    d["tbias"] = f(np.stack([np.stack([t5[b0][:, :, h], t5[b1][:, :, h]]) for h in range(4)]))
    d["tconst"] = f(np.broadcast_to(t5[15][None, :], (128, 4)))
    crb = f(inp["chunk_rel_bias"])
    i0 = np.clip(rel0, -128, 128) + 128; i1 = np.clip(rel1, -128, 128) + 128
    d["cbias"] = f(np.stack([np.stack([np.stack([crb[l][i0][:, :, h], crb[l][i1][:, :, h]]) for h in range(4)]) for l in range(L)]))
    d["cconst"] = f(np.stack([np.broadcast_to(crb[l][0][None, :], (128, 4)) for l in range(L)]))
    d["mqg"] = f(np.asarray(inp["mla_q_norm_g"]).reshape(L, 256, 1))
    d["mkvg"] = f(np.asarray(inp["mla_kv_norm_g"]).reshape(L, 128, 1))
    wuq = f(inp["mla_w_uq"])
    ucols = []
    for h in range(4):
        ucols += list(r(96 * h, 96 * h + 96)) + list(r(96 * h, 96 * h + 64)) + list(r(96 * h + 80, 96 * h + 96)) + list(r(96 * h + 64, 96 * h + 80))
    d["w_uq"] = f(wuq[:, :, np.array(ucols)])
    wukv = f(inp["mla_w_ukv"])
    kc_ = np.concatenate([r(128 * h, 128 * h + 64) for h in range(4)] + [r(128 * h + 64, 128 * h + 128) for h in range(4)])
    d["w_ukv"] = f(wukv[:, :, kc_])
    d["ropet"] = rope_table(S, 0)
    d["w_gate"] = f(inp["w_gate"]); d["b_gate"] = f(inp["b_gate"]); d["w_branch"] = f(inp["w_branch"]); d["w_out"] = f(inp["w_out"])
    d["ln1"] = f(np.stack([np.stack([np.broadcast_to(inp["ln1_g"][l], (128, D)), np.broadcast_to(inp["ln1_b"][l], (128, D))]) for l in range(L)]))
    d["ln2"] = f(np.stack([np.stack([np.broadcast_to(inp["ln2_g"][l], (128, D)), np.broadcast_to(inp["ln2_b"][l], (128, D))]) for l in range(L)]))
    d["wr"] = f(np.concatenate([inp["router_group_w"], inp["router_expert_w"]], axis=2))
    d["br"] = f(np.concatenate([inp["router_group_b"], inp["router_expert_b"]], axis=1).reshape(L, 1, 36))
    d["ew1"] = f(inp["expert_w1"]); d["ew3"] = f(inp["expert_w3"]); d["ew2"] = f(inp["expert_w2"])
    d["ident"] = np.eye(128, dtype=np.float32)
    d["trimask"] = (kq[:, None] <= kq[None, :]).astype(np.float32)
    return d


def rope_table(S, pos0):
    inv_freq = np.power(np.float32(10000.0), -np.arange(16, dtype=np.float32) * np.float32(2.0) / np.float32(32))
    pos = np.maximum(np.arange(S, dtype=np.float32) + np.float32(pos0), np.float32(0))
    ang = pos[:, None] * inv_freq[None, :].astype(np.float32)
    cs, sn = np.cos(ang).astype(np.float32).T, np.sin(ang).astype(np.float32).T
    return np.ascontiguousarray(np.stack([np.concatenate([cs, cs]), np.concatenate([-sn, sn])]).astype(np.float32))


def make_in_maps(inputs, S, nbatch):
    w = prep_weights(inputs, S)
    x = np.asarray(inputs["x"], dtype=np.float32)
    rope0 = rope_table(S, -128)
    maps = []
    for c in range(2 * nbatch):
        b, p = c // 2, c % 2
        m = dict(w)
        if p == 1:
            m["xin"] = np.ascontiguousarray(x[b, :S])
            m["keep"] = np.ones((128, 1), np.float32)
        else:
            m["xin"] = np.ascontiguousarray(np.concatenate([np.zeros((128, D), np.float32), x[b, :S - 128]], axis=0))
            m["keep"] = np.zeros((128, 1), np.float32)
            m["ropet"] = rope0
        maps.append(m)
    return maps


def assemble(results, S, nbatch):
    out = np.empty((nbatch, S, D), np.float32)
    nown = S // 256
    for c in range(2 * nbatch):
        b, p = c // 2, c % 2
        o = np.asarray(results[c]["out"], dtype=np.float32).reshape(nown, 128, D)
        ov = out[b].reshape(S // 128, 128, D)
        if p == 1:
            ov[1::2] = o
        else:
            ov[0::2] = o
    return out


def own_config(S):
    nb = S // 128
    return {0: list(range(nb)), 1: list(range(1, nb, 2))}


def kernel(**inputs):
    S, nbatch = 8192, 4
    nc, _ = build_program(S, [0, 1], own_config(S))
    maps = make_in_maps(inputs, S, nbatch)
    res = run_bass_kernel_spmd(nc, maps, core_ids=list(range(2 * nbatch)))
    return assemble(res.results, S, nbatch)
```

```python
import contextlib
import math
import numpy as np
import concourse.bass as bass
import concourse.mybir as mybir
from concourse.bass_utils import run_bass_kernel_spmd

F32 = mybir.dt.float32
BF16 = mybir.dt.bfloat16
AF = mybir.ActivationFunctionType
ALU = mybir.AluOpType
AX = mybir.AxisListType

D = 1024
KC = 8
DEPTH = 2
ALPHA = (2 * DEPTH) ** 0.25
EPS = 1e-5
NEXP = 32
DE = 512
C_DK, C_CK, C_FK, C_MKV, C_MKR, C_MKRS, C_FF = 0, 256, 512, 768, 896, 928, 960
C_Q = 964
C_DQ, C_CQ, C_FQ, C_MQ = C_Q, C_Q + 256, C_Q + 512, C_Q + 768
C_V = C_Q + 1024
WEXT = C_V + 768
MIX = ("diff", "chunk", "mla", "fox")
VCOL = {"diff": 0, "chunk": 256, "fox": 512, "mla": 768}


class Sched:
    ENG = ("pe", "act", "dve", "pool", "sp")
    NDQ = 6

    def __init__(self, nc, es):
        self.nc = nc
        self.eng = {"pe": nc.tensor, "act": nc.scalar, "dve": nc.vector, "pool": nc.gpsimd, "sp": nc.sync}
        self.es = es
        self.semobjs = {}
        self.nsem = 0
        self.ekey = {}
        self.cnt = {}
        for e in self.ENG:
            self._new_eng_sem(e)
        self.seen = {e: {} for e in self.ENG}
        self.dq = {}
        for q in ("sp", "act", "pool"):
            self.dq[q] = {"keys": [self._new_sem(("d", q)) for i in range(self.NDQ)],
                          "cnt": [0] * self.NDQ, "nxt": 0}
        self.lastw = {}
        self.readers = {}
        self.ninst = 0

    LIMIT = 30000

    def _new_sem(self, tag):
        self.nsem += 1
        key = (tag, self.nsem)
        self.semobjs[key] = self.es.enter_context(self.nc.semaphore(f"s{self.nsem}"))
        return key

    def _new_eng_sem(self, e):
        self.ekey[e] = self._new_sem(e)
        self.cnt[e] = 0

    def _semobj(self, key):
        return self.semobjs[key]

    def _wait(self, e, deps):
        need = {}
        for k, v in deps:
            if k[0] == "pe" and e == "pe":
                continue
            if v > need.get(k, 0):
                need[k] = v
        for k, v in need.items():
            if self.seen[e].get(k, 0) >= v:
                continue
            self.eng[e].wait_ge(self._semobj(k), v)
            self.seen[e][k] = v

    def _deps(self, reads, writes):
        deps = []
        for k in reads:
            t = self.lastw.get(k)
            if t:
                deps.append(t)
        for k in writes:
            t = self.lastw.get(k)
            if t:
                deps.append(t)
            deps.extend(self.readers.get(k, ()))
        return deps

    def _record(self, tok, reads, writes):
        for k in reads:
            self.readers.setdefault(k, []).append(tok)
        for k in writes:
            self.lastw[k] = tok
            self.readers[k] = []

    def op(self, e, fn, reads=(), writes=()):
        self._wait(e, self._deps(reads, writes))
        inst = fn(self.eng[e])
        if self.cnt[e] >= self.LIMIT:
            self._new_eng_sem(e)
        self.cnt[e] += 1
        inst.then_inc(self.semobjs[self.ekey[e]], 1)
        self._record((self.ekey[e], self.cnt[e]), reads, writes)
        self.ninst += 1

    def dma(self, q, out, in_, reads=(), writes=(), **kw):
        st = self.dq[q]
        i = st["nxt"]
        st["nxt"] = (i + 1) % self.NDQ
        deps = self._deps(reads, writes)
        if st["cnt"][i]:
            deps.append((st["keys"][i], st["cnt"][i]))
        self._wait(q, deps)
        if st["cnt"][i] >= self.LIMIT:
            st["keys"][i] = self._new_sem(("d", q))
            st["cnt"][i] = 0
        self.eng[q].dma_start(out=out, in_=in_, **kw).then_inc(self.semobjs[st["keys"][i]], 16)
        st["cnt"][i] += 16
        self._record((st["keys"][i], st["cnt"][i]), reads, writes)
        self.ninst += 1

    def _all(self):
        deps = []
        for q, st in self.dq.items():
            for i in range(self.NDQ):
                if st["cnt"][i]:
                    deps.append((st["keys"][i], st["cnt"][i]))
        for e in self.ENG:
            if self.cnt[e]:
                deps.append((self.ekey[e], self.cnt[e]))
        return deps

    def barrier(self):
        deps = self._all()
        for e in self.ENG:
            self._wait(e, [d for d in deps if d[0] != self.ekey[e]])

    def finish(self):
        self.barrier()


def build_program(S, layers, own_by_layer, n_in_layers=DEPTH):
    NB = S // 128
    NG = S // 512
    nc = bass.Bass("TRN2", target_bir_lowering=False)
    L = n_in_layers

    def din(name, shape, dt=F32):
        return nc.dram_tensor(name, list(shape), dt, kind="ExternalInput").ap()

    def dscr(name, shape, dt):
        return nc.dram_tensor(name, list(shape), dt, kind="Internal").ap()

    xin = din("xin", [S, D])
    lnin = din("lnin", [2, 128, D])
    w_ext = din("w_ext", [L, D, WEXT])
    nbf = din("nbf", [L, 4, 1])
    dlam = din("dlam", [L, 128, 128])
    dng = din("dng", [L, 64, 1])
    tbias = din("tbias", [4, 2, 128, 128])
    tconst = din("tconst", [128, 4])
    cbias = din("cbias", [L, 4, 2, 128, 128])
    cconst = din("cconst", [L, 128, 4])
    mqg = din("mqg", [L, 256, 1])
    mkvg = din("mkvg", [L, 128, 1])
    w_uq = din("w_uq", [L, 256, 768])
    w_ukv = din("w_ukv", [L, 128, 512])
    ropet = din("ropet", [2, 32, S])
    w_gate = din("w_gate", [L, 4, D, D])
    b_gate = din("b_gate", [L, 4, D])
    w_branch = din("w_branch", [L, 4, 256, D])
    w_out = din("w_out", [L, D, D])
    ln1 = din("ln1", [L, 2, 128, D])
    ln2 = din("ln2", [L, 2, 128, D])
    wr = din("wr", [L, D, 36])
    br = din("br", [L, 1, 36])
    ew1 = din("ew1", [L, NEXP, D, DE])
    ew3 = din("ew3", [L, NEXP, D, DE])
    ew2 = din("ew2", [L, NEXP, DE, D])
    keep = din("keep", [128, 1])
    ident = din("ident", [128, 128])
    trimask = din("trimask", [128, 128])
    NOmax = max(len(o) for o in own_by_layer.values()) * 128
    NOlast = len(own_by_layer[layers[-1]]) * 128
    out = nc.dram_tensor("out", [NOlast, D], F32, kind="ExternalOutput").ap()

    hres = dscr("hres", [S, D], F32)
    hmid = dscr("hmid", [S, D], F32)
    KT = {m: dscr("KT_" + m, [256, S], BF16) for m in ("diff", "chunk", "fox", "mla")}
    KTr = dscr("KT_mla_rope", [32, S], BF16)
    KC_f = dscr("KC_fox", [4, 3, S], BF16)
    QT = {m: dscr("QT_" + m, [384 if m == "mla" else 256, NOmax], BF16) for m in MIX}
    QC_f = dscr("QC_fox", [4, 3, NOmax], BF16)
    VV = dscr("VV", [16, 128, NB, 65], BF16)
    OT = dscr("OT", [4, 256, NOmax], BF16)
    h1d = dscr("h1d", [NOmax, D], F32)
    h1T = dscr("h1T", [D, NOmax], BF16)
    Gd = dscr("Gd", [NOmax, NEXP], F32)
    dscr_l = {"OTd": dscr("OTd", [2, 256, NOmax], F32)}

    es = contextlib.ExitStack()
    with es:
        sc = Sched(nc, es)

        def sb(name, shape, dt):
            return es.enter_context(nc.sbuf_tensor(name, list(shape), dt))

        PS = [es.enter_context(nc.psum_tensor(f"ps{i}", [128, 512], F32)) for i in range(8)]
        ps_rr = [0, 0]

        ps_pool = [list(range(7))]

        def psget(n=1):
            pool = ps_pool[0]
            if n == 2:
                pairs = [0, 2, 4]
                i = pairs[ps_rr[1] % 3]
                ps_rr[1] += 1
                return i
            i = pool[ps_rr[0] % len(pool)]
            ps_rr[0] += 1
            return i

        def pk(i, n=1):
            return [("ps", i + j) for j in range(n)]

        id_f = sb("id_f", [128, 128], F32)
        id_b = sb("id_b", [128, 128], BF16)
        tri_b = sb("tri_b", [128, 128], BF16)
        ones_f = sb("ones_f", [128, 512], F32)
        ones_b = sb("ones_b", [128, 128], BF16)
        eps_t = sb("eps_t", [128, 1], F32)
        sc.dma("sp", id_f[:], ident[:, :], writes=["id_f"])
        sc.dma("pool", id_b[:], ident[:, :], writes=["id_b"])
        sc.dma("pool", tri_b[:], trimask[:, :], writes=["tri_b"])
        sc.op("pool", lambda e: e.memset(ones_f[:], 1.0), writes=["ones_f"])
        sc.op("pool", lambda e: e.memset(ones_b[:], 1.0), writes=["ones_b"])
        sc.op("pool", lambda e: e.memset(eps_t[:], EPS), writes=["eps_t"])

        def layer_norm(src, skey, gb, gbkey, dst, dkey, tag, dst2=None, d2key=None):
            st = lnw["st"]; mv = lnw["mv"]; rs = lnw["rs"]; xn = lnw["xn"]
            for j in range(2):
                sc.op("dve", lambda e, j=j: e.bn_stats(st[:, j, :], src[:, j * 512:(j + 1) * 512]),
                      reads=[skey], writes=[("lnst", j)])
            sc.op("dve", lambda e: e.bn_aggr(mv[:], st[:].rearrange("p a b -> p (a b)")),
                  reads=[("lnst", 0), ("lnst", 1)], writes=["lnmv"])
            sc.op("act", lambda e: e.activation(rs[:], mv[:, 1:2], AF.Sqrt, bias=eps_t[:], scale=1.0),
                  reads=["lnmv", "eps_t"], writes=["lnrs"])
            sc.op("dve", lambda e: e.reciprocal(rs[:], rs[:]), reads=["lnrs"], writes=["lnrs"])
            sc.op("dve", lambda e: e.tensor_scalar(xn[:], src[:], mv[:, 0:1], rs[:], ALU.subtract, ALU.mult),
                  reads=[skey, "lnmv", "lnrs"], writes=["lnxn"])
            sc.op("pool", lambda e: e.tensor_tensor(xn[:], xn[:], gb[:, 0, :], ALU.mult),
                  reads=["lnxn", gbkey], writes=["lnxn"])
            sc.op("pool", lambda e: e.tensor_tensor(dst, xn[:], gb[:, 1, :], ALU.add),
                  reads=["lnxn", gbkey], writes=[dkey])
            if dst2 is not None:
                sc.op("act", lambda e: e.activation(dst2, dst, AF.Copy), reads=[dkey], writes=[d2key])

        lnw = {"st": sb("ln_st", [128, 2, 6], F32), "mv": sb("ln_mv", [128, 2], F32),
               "rs": sb("ln_rs", [128, 1], F32), "xn": sb("ln_xn", [128, D], F32)}

        for li, l in enumerate(layers):
            own = list(own_by_layer[l])
            NO = len(own) * 128
            ownidx = {b: i for i, b in enumerate(own)}
            src = xin if li == 0 else hmid
            last = (li == len(layers) - 1)
            dst = out if last else hmid
            lam_init = 0.8 - 0.6 * math.exp(-0.3 * l)

            with contextlib.ExitStack() as pa:
                def sba(name, shape, dt):
                    return pa.enter_context(nc.sbuf_tensor(f"A{l}_{name}", list(shape), dt))
                wext = sba("wext", [128, KC, WEXT], BF16)
                for kc in range(KC):
                    for c0_ in range(0, WEXT, 1024):
                        c1_ = min(WEXT, c0_ + 1024)
                        sc.dma("pool", wext[:, kc, c0_:c1_], w_ext[l, kc * 128:(kc + 1) * 128, c0_:c1_], writes=[("wext", kc)])
                wuq = sba("wuq", [128, 2, 768], BF16)
                for c in range(2):
                    sc.dma("pool", wuq[:, c, :], w_uq[l, c * 128:(c + 1) * 128, :], writes=["wuq"])
                wukv = sba("wukv", [128, 512], BF16)
                sc.dma("pool", wukv[:], w_ukv[l], writes=["wukv"])
                gq = sba("gq", [128, 2], F32)
                for c in range(2):
                    sc.dma("sp", gq[:, c:c + 1], mqg[l, c * 128:(c + 1) * 128, :], writes=["gq"])
                gkv = sba("gkv", [128, 1], F32)
                sc.dma("sp", gkv[:], mkvg[l], writes=["gkv"])
                nb4 = sba("nb4", [4, 1], F32)
                sc.dma("sp", nb4[:], nbf[l], writes=["nb4"])
                sc.op("dve", lambda e: e.tensor_scalar(nb4[:], nb4[:], -1.0, None, ALU.mult), reads=["nb4"], writes=["nb4"])
                if l == 0:
                    gbin = sba("gbin", [128, 2, D], F32)
                    sc.dma("sp", gbin[:], lnin.rearrange("a p d -> p a d"), writes=["gbin"])
                xb = [sba(f"xb{i}", [128, D], F32) for i in range(2)]
                hb = [sba(f"hb{i}", [128, D], F32) for i in range(2)]
                hbf = [sba(f"hbf{i}", [128, D], BF16) for i in range(2)]
                hT = [sba(f"hT{i}", [128, KC, 512], BF16) for i in range(2)]
                ev = [sba(f"ev{i}", [128, 512], BF16) for i in range(3)]
                evn = [0]
                vev = [sba(f"vev{i}", [128, 16, 4, 65], BF16) for i in range(2)]
                for i in range(2):
                    sc.op("pool", lambda e, i=i: e.memset(vev[i][:, :, :, 64:65], 1.0), writes=[("vev", i)])
                cqs = sba("cqs", [128, 2, 512], F32)
                ckvs = sba("ckvs", [128, 512], F32)
                sq = sba("sq", [128, 512], F32)
                rstd = sba("rstd", [128, 512], F32)
                cqn = sba("cqn", [128, 2, 512], BF16)
                ckvn = sba("ckvn", [128, 512], BF16)
                ropeA = sba("ropeA", [32, 2, 512], F32)
                ropeB = sba("ropeB", [96, 2, 512], F32)
                rt1 = sba("rt1", [96, 512], F32)
                rt2 = sba("rt2", [96, 512], F32)
                ffe = sba("ffe", [4, 512], F32)
                cum = [sba(f"cum{i}", [4, 512], F32) for i in range(2)]
                csp = sba("csp", [4, 3, 512], BF16)
                csn = sba("csn", [4, 3, 512], BF16)
                cr = sba("cr", [4, 512], F32)
                sc.op("pool", lambda e: e.memset(cum[1][:], 0.0), writes=[("cum", 1)])

                for g in range(NG):
                    hTg = hT[g % 2]
                    hk = ("hT", g % 2)
                    opos = [j for j in range(4) if (4 * g + j) in ownidx]
                    for j in range(4):
                        b = 4 * g + j
                        i2 = b % 2
                        sc.dma("sp", xb[i2][:], src[b * 128:(b + 1) * 128, :], reads=["src"], writes=[("xb", i2)])
                        if l == 0 and li == 0:
                            layer_norm(xb[i2], ("xb", i2), gbin, "gbin", hb[i2][:], ("hb", i2), "in",
                                       dst2=hbf[i2][:], d2key=("hbf", i2))
                            sc.dma("sp", hres[b * 128:(b + 1) * 128, :], hb[i2][:], reads=[("hb", i2)], writes=["hres"])
                        else:
                            sc.op("act", lambda e, i2=i2: e.activation(hbf[i2][:], xb[i2][:], AF.Copy),
                                  reads=[("xb", i2)], writes=[("hbf", i2)])
                        pt = 7
                        ptv = PS[pt][:].bitcast(BF16)
                        for kc in range(KC):
                            sc.op("pe", lambda e, kc=kc, i2=i2: e.transpose(ptv[:, kc * 128:(kc + 1) * 128],
                                                                           hbf[i2][:, kc * 128:(kc + 1) * 128], id_b[:]),
                                  reads=[("hbf", i2), "id_b"], writes=pk(pt))
                        sc.op("dve", lambda e, j=j: e.tensor_copy(hTg[:, :, j * 128:(j + 1) * 128],
                                                                  ptv.rearrange("p (k t) -> p k t", k=KC)),
                              reads=pk(pt), writes=[hk])

                    def fm(c0, n, cols=None, ncols=512):
                        p = psget()
                        rhs_of = (lambda kc: hTg[:, kc, :]) if cols is None else cols
                        for kc in range(KC):
                            sc.op("pe", lambda e, kc=kc: e.matmul(PS[p][0:n, 0:ncols], lhsT=wext[:, kc, c0:c0 + n],
                                                                   rhs=rhs_of(kc), start=(kc == 0), stop=(kc == KC - 1)),
                                  reads=[hk, ("wext", kc)], writes=pk(p))
                        return p

                    def evac_store(p, n, dram_ap, dkey, scale=None, ncols=512, eng="act"):
                        t = ev[evn[0] % 3]; tk = ("ev", evn[0] % 3); evn[0] += 1
                        if eng == "act":
                            sc.op("act", lambda e: e.activation(t[0:n, 0:ncols], PS[p][0:n, 0:ncols], AF.Copy,
                                                                scale=(1.0 if scale is None else scale)),
                                  reads=pk(p), writes=[tk])
                        else:
                            sc.op("dve", lambda e: e.tensor_copy(t[0:n, 0:ncols], PS[p][0:n, 0:ncols]),
                                  reads=pk(p), writes=[tk])
                        sc.dma("sp", dram_ap, t[0:n, 0:ncols], reads=[tk], writes=[dkey])

                    tsl = slice(g * 512, (g + 1) * 512)
                    for (mname, c0) in (("diff", C_DK), ("chunk", C_CK), ("fox", C_FK)):
                        for hh in range(2):
                            p = fm(c0 + hh * 128, 128)
                            evac_store(p, 128, KT[mname][hh * 128:(hh + 1) * 128, tsl], "KT_" + mname,
                                       eng=("act" if hh == 0 else "dve"))
                    if len(opos) == 4:
                        ocols = None; nq = 512
                    elif len(opos) == 2 and opos[1] - opos[0] == 2:
                        par = opos[0]
                        ocols = (lambda kc: hTg[:, kc, :].rearrange("p (b two t) -> p b two t", two=2, t=128)[:, :, par, :])
                        nq = 256
                    elif len(opos) == 0:
                        ocols = None; nq = 0
                    else:
                        raise NotImplementedError(opos)
                    if nq:
                        oq0 = ownidx[4 * g + opos[0]] * 128
                        qsl = slice(oq0, oq0 + nq)
                        for (mname, c0, dh) in (("diff", C_DQ, 32), ("chunk", C_CQ, 64), ("fox", C_FQ, 64)):
                            for hh in range(2):
                                p = fm(c0 + hh * 128, 128, cols=ocols, ncols=nq)
                                evac_store(p, 128, QT[mname][hh * 128:(hh + 1) * 128, qsl], "QT_" + mname,
                                           scale=dh ** -0.5, ncols=nq, eng="act")
                    p = fm(C_FF, 4)
                    cg = cum[g % 2]; cprev = cum[(g + 1) % 2]
                    sc.op("act", lambda e: e.activation(ffe[:], PS[p][0:4, :], AF.Exp, bias=nb4[:], scale=-1.0),
                          reads=pk(p) + ["nb4"], writes=["ffe"])
                    sc.op("act", lambda e: e.activation(ffe[:], ffe[:], AF.Ln, bias=1.0, scale=1.0),
                          reads=["ffe"], writes=["ffe"])
                    sc.op("dve", lambda e: e.tensor_tensor_scan(cg[:], ones_f[0:4, :], ffe[:], cprev[:, 511:512],
                                                                 ALU.mult, ALU.subtract),
                          reads=["ffe", "ones_f", ("cum", (g + 1) % 2)], writes=[("cum", g % 2)])
                    sc.op("dve", lambda e: e.tensor_copy(csp[:, 0, :], cg[:]), reads=[("cum", g % 2)], writes=["csp"])
                    sc.op("dve", lambda e: e.tensor_tensor(cr[:], cg[:], csp[:, 0, :], ALU.subtract),
                          reads=[("cum", g % 2), "csp"], writes=["cr"])
                    sc.op("dve", lambda e: e.tensor_copy(csp[:, 1, :], cr[:]), reads=["cr"], writes=["csp"])
                    sc.op("dve", lambda e: e.tensor_tensor(cr[:], cr[:], csp[:, 1, :], ALU.subtract),
                          reads=["cr", "csp"], writes=["cr"])
                    sc.op("dve", lambda e: e.tensor_copy(csp[:, 2, :], cr[:]), reads=["cr"], writes=["csp"])
                    sc.op("dve", lambda e: e.tensor_scalar(csn[:], csp[:], -1.0, None, ALU.mult), reads=["csp"], writes=["csn"])
                    sc.dma("sp", KC_f[:, :, tsl], csn[:], reads=["csn"], writes=["KC_f"])
                    for jj in opos:
                        o0 = ownidx[4 * g + jj] * 128
                        sc.dma("sp", QC_f[:, :, o0:o0 + 128], csp[:, :, jj * 128:(jj + 1) * 128], reads=["csp"], writes=["QC_f"])
                    sc.dma("sp", ropeA[:], ropet[:, :, tsl].rearrange("a r t -> r a t"), writes=["ropeA"])
                    sc.dma("sp", ropeB[64:96, :, :], ropet[:, :, tsl].rearrange("a r t -> r a t"), writes=["ropeB"])
                    p = fm(C_MKV, 128)
                    sc.op("act", lambda e: e.activation(ckvs[:], PS[p][:, :], AF.Copy), reads=pk(p), writes=["ckvs"])
                    sc.op("act", lambda e: e.activation(sq[:], PS[p][:, :], AF.Square), reads=pk(p), writes=["sq"])
                    p2 = psget()
                    sc.op("pe", lambda e: e.matmul(PS[p2][:, :], lhsT=ones_f[:, 0:128], rhs=sq[:], start=True, stop=True),
                          reads=["sq", "ones_f"], writes=pk(p2))
                    sc.op("act", lambda e: e.activation(rstd[:], PS[p2][:, :], AF.Sqrt, bias=eps_t[:], scale=1.0 / 128),
                          reads=pk(p2) + ["eps_t"], writes=["rstd"])
                    sc.op("dve", lambda e: e.reciprocal(rstd[:], rstd[:]), reads=["rstd"], writes=["rstd"])
                    sc.op("dve", lambda e: e.scalar_tensor_tensor(ckvn[:], ckvs[:], gkv[:, 0:1], rstd[:], ALU.mult, ALU.mult),
                          reads=["ckvs", "gkv", "rstd"], writes=["ckvn"])
                    for hh in range(2):
                        p = psget()
                        sc.op("pe", lambda e, hh=hh: e.matmul(PS[p][:, :], lhsT=wukv[:, hh * 128:(hh + 1) * 128], rhs=ckvn[:],
                                                               start=True, stop=True), reads=["wukv", "ckvn"], writes=pk(p))
                        evac_store(p, 128, KT["mla"][hh * 128:(hh + 1) * 128, tsl], "KT_mla", eng=("act" if hh else "dve"))
                    pA = fm(C_MKR, 32); pB = fm(C_MKRS, 32)
                    sc.op("dve", lambda e: e.tensor_tensor(rt1[0:32, :], PS[pA][0:32, :], ropeA[:, 0, :], ALU.mult),
                          reads=pk(pA) + ["ropeA"], writes=["rt1"])
                    sc.op("dve", lambda e: e.tensor_tensor(rt2[0:32, :], PS[pB][0:32, :], ropeA[:, 1, :], ALU.mult),
                          reads=pk(pB) + ["ropeA"], writes=["rt2"])
                    t = ev[evn[0] % 3]; tk = ("ev", evn[0] % 3); evn[0] += 1
                    sc.op("dve", lambda e: e.tensor_tensor(t[0:32, :], rt1[0:32, :], rt2[0:32, :], ALU.add),
                          reads=["rt1", "rt2"], writes=[tk])
                    sc.dma("sp", KTr[:, tsl], t[0:32, :], reads=[tk], writes=["KTr"])
                    if nq:
                        for c in range(2):
                            p = fm(C_MQ + c * 128, 128, cols=ocols, ncols=nq)
                            sc.op("act", lambda e, c=c: e.activation(cqs[:, c, 0:nq], PS[p][:, 0:nq], AF.Copy),
                                  reads=pk(p), writes=[("cqs", c)])
                        p2 = psget()
                        for c in range(2):
                            sc.op("act", lambda e, c=c: e.activation(sq[:, 0:nq], cqs[:, c, 0:nq], AF.Square),
                                  reads=[("cqs", c)], writes=["sq"])
                            sc.op("pe", lambda e, c=c: e.matmul(PS[p2][:, 0:nq], lhsT=ones_f[:, 0:128], rhs=sq[:, 0:nq],
                                                                 start=(c == 0), stop=(c == 1)),
                                  reads=["sq", "ones_f"], writes=pk(p2))
                        sc.op("act", lambda e: e.activation(rstd[:, 0:nq], PS[p2][:, 0:nq], AF.Sqrt, bias=eps_t[:], scale=1.0 / 256),
                              reads=pk(p2) + ["eps_t"], writes=["rstd"])
                        sc.op("dve", lambda e: e.reciprocal(rstd[:, 0:nq], rstd[:, 0:nq]), reads=["rstd"], writes=["rstd"])
                        for c in range(2):
                            sc.op("dve", lambda e, c=c: e.scalar_tensor_tensor(cqn[:, c, 0:nq], cqs[:, c, 0:nq], gq[:, c:c + 1],
                                                                                rstd[:, 0:nq], ALU.mult, ALU.mult),
                                  reads=[("cqs", c), "gq", "rstd"], writes=["cqn"])
                        qscale = 96 ** -0.5
                        for h in range(4):
                            pN = psget(); pS_ = psget()
                            for c in range(2):
                                sc.op("pe", lambda e, c=c, h=h: e.matmul(PS[pN][0:96, 0:nq], lhsT=wuq[:, c, h * 192:h * 192 + 96],
                                                                          rhs=cqn[:, c, 0:nq], start=(c == 0), stop=(c == 1)),
                                      reads=["wuq", "cqn"], writes=pk(pN))
                            for c in range(2):
                                sc.op("pe", lambda e, c=c, h=h: e.matmul(PS[pS_][0:96, 0:nq], lhsT=wuq[:, c, h * 192 + 96:h * 192 + 192],
                                                                          rhs=cqn[:, c, 0:nq], start=(c == 0), stop=(c == 1)),
                                      reads=["wuq", "cqn"], writes=pk(pS_))
                            t = ev[evn[0] % 3]; tk = ("ev", evn[0] % 3); evn[0] += 1
                            sc.op("act", lambda e, t=t: e.activation(t[0:64, 0:nq], PS[pN][0:64, 0:nq], AF.Copy, scale=qscale),
                                  reads=pk(pN), writes=[tk])
                            if ocols is None:
                                rc = ropeB[64:96, 0, :]; rs_ = ropeB[64:96, 1, :]
                            else:
                                rc = ropeB[64:96, 0, :].rearrange("p (b two t) -> p b two t", two=2, t=128)[:, :, par, :]
                                rs_ = ropeB[64:96, 1, :].rearrange("p (b two t) -> p b two t", two=2, t=128)[:, :, par, :]
                            def v3(ap):
                                return ap if ocols is None else ap.rearrange("p (b t) -> p b t", t=128)
                            sc.op("dve", lambda e: e.scalar_tensor_tensor(v3(rt1[64:96, 0:nq]), v3(PS[pN][64:96, 0:nq]), qscale, rc,
                                                                          ALU.mult, ALU.mult),
                                  reads=pk(pN) + ["ropeB"], writes=["rt1"])
                            sc.op("dve", lambda e: e.scalar_tensor_tensor(v3(rt2[64:96, 0:nq]), v3(PS[pS_][64:96, 0:nq]), qscale, rs_,
                                                                          ALU.mult, ALU.mult),
                                  reads=pk(pS_) + ["ropeB"], writes=["rt2"])
                            sc.op("dve", lambda e, t=t: e.tensor_tensor(t[64:96, 0:nq], rt1[64:96, 0:nq], rt2[64:96, 0:nq], ALU.add),
                                  reads=["rt1", "rt2", tk], writes=[tk])
                            sc.dma("sp", QT["mla"][h * 96:(h + 1) * 96, qsl], t[0:96, 0:nq], reads=[tk], writes=["QT_mla"])
                    vt = vev[g % 2]; vk = ("vev", g % 2)
                    for j in range(4):
                        pa_ = psget(); pb_ = psget()
                        for kc in range(KC):
                            sc.op("pe", lambda e, kc=kc: e.matmul(PS[pa_][:, :], lhsT=hTg[:, kc, j * 128:(j + 1) * 128],
                                                                   rhs=wext[:, kc, C_V:C_V + 512], start=(kc == 0), stop=(kc == KC - 1)),
                                  reads=[hk, ("wext", kc)], writes=pk(pa_))
                        for kc in range(KC):
                            sc.op("pe", lambda e, kc=kc: e.matmul(PS[pb_][:, 0:256], lhsT=hTg[:, kc, j * 128:(j + 1) * 128],
                                                                   rhs=wext[:, kc, C_V + 512:C_V + 768], start=(kc == 0), stop=(kc == KC - 1)),
                                  reads=[hk, ("wext", kc)], writes=pk(pb_))
                        sc.op("act", lambda e: e.activation(vt[:, 0:8, j, 0:64], PS[pa_][:, :].rearrange("p (a e) -> p a e", e=64), AF.Copy),
                              reads=pk(pa_), writes=[vk])
                        sc.op("dve", lambda e: e.tensor_copy(vt[:, 8:12, j, 0:64], PS[pb_][:, 0:256].rearrange("p (a e) -> p a e", e=64)),
                              reads=pk(pb_), writes=[vk])
                        pc_ = psget()
                        sc.op("pe", lambda e: e.matmul(PS[pc_][:, 0:256], lhsT=ckvn[:, j * 128:(j + 1) * 128], rhs=wukv[:, 256:512],
                                                       start=True, stop=True), reads=["ckvn", "wukv"], writes=pk(pc_))
                        sc.op("act", lambda e: e.activation(vt[:, 12:16, j, 0:64], PS[pc_][:, 0:256].rearrange("p (a e) -> p a e", e=64), AF.Copy),
                              reads=pk(pc_), writes=[vk])
                    sc.dma("sp", VV[:, :, 4 * g:4 * g + 4, :].rearrange("a p b e -> p a b e"), vt[:], reads=[vk], writes=["VV"])
                sc.barrier()
            if PHASES < 2:
                continue
            hsrc = hres if (l == 0 and li == 0) else src

            with contextlib.ExitStack() as pb:
                def sbb(name, shape, dt):
                    return pb.enter_context(nc.sbuf_tensor(f"B{l}_{name}", list(shape), dt))
                NOB = len(own)
                ps_pool[0] = [0, 1, 2, 3, 4]
                pon = [0]
                ktb = [sbb(f"kt{i}", [96, S], BF16) for i in range(2)]
                qtb = [sbb(f"qt{i}", [96, NO], BF16) for i in range(2)]
                vtb = [sbb(f"vt{i}", [128, NB, 65], BF16) for i in range(2)]
                ptb = [sbb(f"pt{i}", [128, 512], BF16) for i in range(3)]
                btile = sbb("btile", [128, 4, 2, 128], BF16)
                bstage = sbb("bstage", [128, 4, 2, 128], F32)
                bconst = sbb("bconst", [128, 4], F32)
                keept = sbb("keept", [128, 1], F32)
                sc.dma("sp", keept[:], keep[:, :], writes=["keept"])
                zero_c = sbb("zero_c", [128, 1], F32)
                sc.op("pool", lambda e: e.memset(zero_c[:], 0.0), writes=["zero_c"])
                osb = sbb("osb", [64, 512], F32)
                osb2 = sbb("osb2", [64, 512], F32)
                rden = sbb("rden", [1, 512], F32)
                rdb = sbb("rdb", [64, 512], F32)
                obf = [sbb(f"obf{i}", [64, 512], BF16) for i in range(2)]
                obf32 = [sbb(f"obf32_{i}", [64, 512], F32) for i in range(2)]
                ptn = [0]; otn = [0]
                osq = sbb("osq", [64, 512], F32)
                lamt = sbb("lamt", [128, 128], F32)
                lamv = sbb("lamv", [128, 4], F32)
                dgn = sbb("dgn", [64, 1], F32)
                sc.dma("sp", lamt[:], dlam[l], writes=["lamt"])
                sc.dma("sp", dgn[:], dng[l], writes=["dgn"])
                sc.op("dve", lambda e: e.tensor_tensor(lamt[:, 0:32], lamt[:, 0:32], lamt[:, 32:64], ALU.mult), reads=["lamt"], writes=["lamt"])
                sc.op("dve", lambda e: e.tensor_tensor(lamt[:, 64:96], lamt[:, 64:96], lamt[:, 96:128], ALU.mult), reads=["lamt"], writes=["lamt"])
                sc.op("dve", lambda e: e.tensor_reduce(lamv[:, 0:1], lamt[:, 0:32], AX.X, ALU.add), reads=["lamt"], writes=["lamv"])
                sc.op("dve", lambda e: e.tensor_reduce(lamv[:, 1:2], lamt[:, 64:96], AX.X, ALU.add), reads=["lamt"], writes=["lamv"])
                sc.op("act", lambda e: e.activation(lamv[:, 0:2], lamv[:, 0:2], AF.Exp), reads=["lamv"], writes=["lamv"])
                sc.op("dve", lambda e: e.tensor_tensor(lamv[:, 2:3], lamv[:, 0:1], lamv[:, 1:2], ALU.subtract), reads=["lamv"], writes=["lamv"])
                sc.op("dve", lambda e: e.tensor_scalar(lamv[:, 3:4], lamv[:, 2:3], lam_init, -1.0, ALU.add, ALU.mult), reads=["lamv"], writes=["lamv"])
                sc.op("dve", lambda e: e.tensor_scalar(dgn[:], dgn[:], 1.0 - lam_init, None, ALU.mult), reads=["dgn"], writes=["dgn"])

                def load_bias(dram_tiles, dram_const):
                    sc.dma("sp", bstage[:], dram_tiles.rearrange("h a k q -> k h a q"), writes=["bstage"])
                    sc.dma("sp", bconst[:], dram_const, writes=["bconst"])
                    for h in range(4):
                        sc.op("dve", lambda e, h=h: e.tensor_scalar(btile[:, h, :, :], bstage[:, h, :, :], bconst[:, h:h + 1], None, ALU.subtract),
                              reads=["bstage", "bconst"], writes=["btile"])

                OTd = dscr_l["OTd"]
                ldn = [0]
                for mi, m in enumerate(MIX):
                    if m not in DBG_MIX:
                        continue
                    if m == "diff":
                        load_bias(tbias, tconst)
                    elif m == "chunk":
                        load_bias(cbias[l], cconst[l])
                    dqk = {"diff": 32, "chunk": 64, "mla": 96, "fox": 70}[m]
                    QB = 1 if m == "chunk" else 4
                    nstream = 2 if m == "diff" else 1
                    tiles = [own[i:i + QB] for i in range(0, NOB, QB)]
                    for h in range(4):
                        vi = h % 2
                        sc.dma(BQ, vtb[vi][:], VV[VCOL[m] // 64 + h], reads=["VV"], writes=[("vt", vi)])
                        sc.op("dve", lambda e: e.tensor_scalar(vtb[vi][:, 0, :], vtb[vi][:, 0, :], keept[:, 0:1], None, ALU.mult),
                              reads=[("vt", vi), "keept"], writes=[("vt", vi)])
                        for s_ in range(nstream):
                            bi = ldn[0] % 2; ldn[0] += 1
                            kt = ktb[bi]; qt = qtb[bi]; kk = ("kt", bi); qk = ("qt", bi)
                            if m == "diff":
                                r0 = h * 64 + s_ * 32
                                sc.dma(BQ, kt[0:32, :], KT[m][r0:r0 + 32, :], reads=["KT_" + m], writes=[kk])
                                sc.dma(BQ, qt[0:32, :], QT[m][r0:r0 + 32, 0:NO], reads=["QT_" + m], writes=[qk])
                            elif m == "chunk":
                                sc.dma(BQ, kt[0:64, :], KT[m][h * 64:h * 64 + 64, :], reads=["KT_" + m], writes=[kk])
                                sc.dma(BQ, qt[0:64, :], QT[m][h * 64:h * 64 + 64, 0:NO], reads=["QT_" + m], writes=[qk])
                            elif m == "mla":
                                sc.dma(BQ, kt[0:64, :], KT[m][h * 64:h * 64 + 64, :], reads=["KT_" + m], writes=[kk])
                                sc.dma(BQ, kt[64:96, :], KTr[:, :], reads=["KTr"], writes=[kk])
                                sc.dma(BQ, qt[0:96, :], QT[m][h * 96:h * 96 + 96, 0:NO], reads=["QT_" + m], writes=[qk])
                            else:
                                sc.op("pool", lambda e, kt=kt: e.memset(kt[64:70, :], 1.0), writes=[kk])
                                sc.op("pool", lambda e, qt=qt: e.memset(qt[64:70, :], 1.0), writes=[qk])
                                sc.dma(BQ, kt[0:64, :], KT[m][h * 64:h * 64 + 64, :], reads=["KT_" + m], writes=[kk])
                                sc.dma(BQ, kt[67:70, :], KC_f[h, :, :], reads=["KC_f"], writes=[kk])
                                sc.dma(BQ, qt[0:64, :], QT[m][h * 64:h * 64 + 64, 0:NO], reads=["QT_" + m], writes=[qk])
                                sc.dma(BQ, qt[64:67, :], QC_f[h, :, 0:NO], reads=["QC_f"], writes=[qk])
                            for ti, blks in enumerate(tiles):
                                nqb = len(blks)
                                q0 = ti * QB * 128
                                lo = [max(0, bq - 4) if m == "chunk" else 0 for bq in blks]
                                kbs = list(range(min(lo), blks[-1] + 1))
                                po = 5 + (pon[0] % 2); pon[0] += 1
                                started = [False] * nqb

                                def near(kb, j):
                                    if m not in ("diff", "chunk"):
                                        return None
                                    if kb == blks[j]:
                                        return 0
                                    if kb == blks[j] - 1:
                                        return 1
                                    return None

                                def emit_qk(kb):
                                    js = [j for j in range(nqb) if lo[j] <= kb <= blks[j]]
                                    ja, jb = js[0], js[-1]
                                    p = psget()
                                    ksl = kt[0:dqk, kb * 128:(kb + 1) * 128]
                                    runs = []
                                    j = ja
                                    while j <= jb:
                                        kind = near(kb, j)
                                        if kind is not None:
                                            runs.append((j, j, kind)); j += 1
                                        else:
                                            j2 = j
                                            while j2 + 1 <= jb and near(kb, j2 + 1) is None:
                                                j2 += 1
                                            runs.append((j, j2, None)); j = j2 + 1
                                    for (a, b_, kind) in runs:
                                        cs = slice(a * 128, (b_ + 1) * 128)
                                        sc.op("pe", lambda e, cs=cs, kind=kind: e.matmul(PS[p][:, cs], lhsT=ksl, rhs=qt[0:dqk, q0 + cs.start:q0 + cs.stop],
                                                                                           start=True, stop=(kind is None)),
                                              reads=[kk, qk], writes=pk(p))
                                        if kind is not None:
                                            sc.op("pe", lambda e, cs=cs, kind=kind: e.matmul(PS[p][:, cs], lhsT=id_b[:], rhs=btile[:, h, kind, :],
                                                                                               start=False, stop=True),
                                                  reads=["id_b", "btile"], writes=pk(p))
                                    return (kb, p, ja, jb)

                                def emit_rest(item):
                                    kb, p, ja, jb = item
                                    cs = slice(ja * 128, (jb + 1) * 128)
                                    pi = ptn[0] % 3; ptn[0] += 1
                                    pt = ptb[pi]; ptk = ("pt", pi)
                                    bias_ap = bconst[:, h:h + 1] if m in ("diff", "chunk") else zero_c[:]
                                    sc.op("act", lambda e: e.activation(pt[:, cs], PS[p][:, cs], AF.Exp, bias=bias_ap, scale=1.0),
                                          reads=pk(p) + ["bconst", "zero_c"], writes=[ptk])
                                    for j in range(ja, jb + 1):
                                        c0 = j * 128
                                        if kb == blks[j]:
                                            if m == "fox":
                                                sc.op("pool", lambda e, c0=c0: e.tensor_tensor(pt[:, c0:c0 + 128], pt[:, c0:c0 + 128], tri_b[:], ALU.mult),
                                                      reads=[ptk, "tri_b"], writes=[ptk])
                                            else:
                                                sc.op("pool", lambda e, c0=c0: e.memset(pt[64:128, c0:c0 + 64], 0.0), writes=[ptk])
                                        if m == "chunk" and kb == blks[j] - 4:
                                            sc.op("pool", lambda e, c0=c0: e.memset(pt[0:64, c0 + 64:c0 + 128], 0.0), writes=[ptk])
                                    j = ja
                                    while j <= jb:
                                        j2 = j
                                        while j2 + 1 <= jb and started[j2 + 1] == started[j]:
                                            j2 += 1
                                        c2 = slice(j * 128, (j2 + 1) * 128)
                                        is_last = (kb == kbs[-1])
                                        sc.op("pe", lambda e, c2=c2, st=(not started[j]), is_last=is_last: e.matmul(
                                            PS[po][0:65, c2], lhsT=vtb[vi][:, kb, :], rhs=pt[:, c2], start=st, stop=is_last),
                                            reads=[("vt", vi), ptk], writes=pk(po))
                                        for jj in range(j, j2 + 1):
                                            started[jj] = True
                                        j = j2 + 1

                                pend = emit_qk(kbs[0])
                                for kb in kbs[1:]:
                                    nxt = emit_qk(kb)
                                    emit_rest(pend)
                                    pend = nxt
                                emit_rest(pend)
                                ncol = nqb * 128
                                sc.op("dve", lambda e: e.tensor_scalar(rden[0:1, 0:ncol], PS[po][64:65, 0:ncol], 1e-30, None, ALU.max), reads=pk(po), writes=["rden"])
                                sc.op("dve", lambda e: e.reciprocal(rden[0:1, 0:ncol], rden[0:1, 0:ncol]), reads=["rden"], writes=["rden"])
                                pbk = 7
                                sc.op("pe", lambda e: e.matmul(PS[pbk][0:64, 0:ncol], lhsT=ones_f[0:1, 0:64], rhs=rden[0:1, 0:ncol], start=True, stop=True),
                                      reads=["rden", "ones_f"], writes=pk(pbk))
                                sc.op("act", lambda e: e.activation(rdb[:, 0:ncol], PS[pbk][0:64, 0:ncol], AF.Copy), reads=pk(pbk), writes=["rdb"])
                                osl = slice(q0, q0 + ncol)
                                oi = otn[0] % 2; otn[0] += 1
                                if m != "diff":
                                    ob = obf[oi]; obk = ("obf", oi)
                                    tgt = OT[mi, h * 64:(h + 1) * 64, osl]; tk_ = "OT"
                                else:
                                    ob = obf32[oi]; obk = ("obf32", oi)
                                    tgt = OTd[s_, h * 64:(h + 1) * 64, osl]; tk_ = "OTd"
                                sc.op("dve", lambda e: e.tensor_tensor(ob[:, 0:ncol], PS[po][0:64, 0:ncol], rdb[:, 0:ncol], ALU.mult),
                                      reads=pk(po) + ["rdb"], writes=[obk])
                                sc.dma("sp", tgt, ob[:, 0:ncol], reads=[obk], writes=[tk_])
                    if m == "diff":
                        for h in range(4):
                            for c0 in range(0, NO, 512):
                                ncol = min(512, NO - c0)
                                sc.dma(BQ, osb[:, 0:ncol], OTd[0, h * 64:(h + 1) * 64, c0:c0 + ncol], reads=["OTd"], writes=["osb"])
                                sc.dma(BQ, osb2[:, 0:ncol], OTd[1, h * 64:(h + 1) * 64, c0:c0 + ncol], reads=["OTd"], writes=["osb2"])
                                sc.op("dve", lambda e: e.scalar_tensor_tensor(osb[:, 0:ncol], osb2[:, 0:ncol], lamv[0:64, 3:4], osb[:, 0:ncol], ALU.mult, ALU.add),
                                      reads=["osb", "osb2", "lamv"], writes=["osb"])
                                sc.op("act", lambda e: e.activation(osq[:, 0:ncol], osb[:, 0:ncol], AF.Square), reads=["osb"], writes=["osq"])
                                pq = 7
                                sc.op("pe", lambda e: e.matmul(PS[pq][0:64, 0:ncol], lhsT=ones_f[0:64, 0:64], rhs=osq[:, 0:ncol], start=True, stop=True),
                                      reads=["osq", "ones_f"], writes=pk(pq))
                                sc.op("act", lambda e: e.activation(rdb[:, 0:ncol], PS[pq][0:64, 0:ncol], AF.Sqrt, bias=eps_t[0:64, :], scale=1.0 / 64),
                                      reads=pk(pq) + ["eps_t"], writes=["rdb"])
                                sc.op("dve", lambda e: e.reciprocal(rdb[:, 0:ncol], rdb[:, 0:ncol]), reads=["rdb"], writes=["rdb"])
                                oi = otn[0] % 2; otn[0] += 1
                                ob = obf[oi]; obk = ("obf", oi)
                                sc.op("dve", lambda e: e.scalar_tensor_tensor(ob[:, 0:ncol], osb[:, 0:ncol], dgn[:, 0:1], rdb[:, 0:ncol], ALU.mult, ALU.mult),
                                      reads=["osb", "dgn", "rdb"], writes=[obk])
                                sc.dma("sp", OT[0, h * 64:(h + 1) * 64, c0:c0 + ncol], ob[:, 0:ncol], reads=[obk], writes=["OT"])
                ps_pool[0] = list(range(7))
                sc.barrier()
            if PHASES < 3:
                continue
            with contextlib.ExitStack() as pcx:
                def sbc(name, shape, dt):
                    return pcx.enter_context(nc.sbuf_tensor(f"C{l}_{name}", list(shape), dt))
                wg = sbc("wg", [128, 4, KC, D], BF16)
                for n in range(4):
                    for kc in range(KC):
                        sc.dma("pool", wg[:, n, kc, :], w_gate[l, n, kc * 128:(kc + 1) * 128, :], writes=["wg"])
                bg = sbc("bg", [1, 4, D], BF16)
                bgf = sbc("bgf", [1, D], F32)
                for n in range(4):
                    sc.dma("sp", bgf[:], b_gate[l, n:n + 1, :], writes=["bgf"])
                    sc.op("dve", lambda e, n=n: e.tensor_copy(bg[0:1, n, :], bgf[:]), reads=["bgf"], writes=["bg"])
                wb = sbc("wb", [128, 4, 2, D], BF16)
                for n in range(4):
                    for c in range(2):
                        sc.dma("pool", wb[:, n, c, :], w_branch[l, n, c * 128:(c + 1) * 128, :], writes=["wb"])
                wo = sbc("wo", [128, KC, D], BF16)
                for kc in range(KC):
                    sc.dma("pool", wo[:, kc, :], w_out[l, kc * 128:(kc + 1) * 128, :], writes=["wo"])
                g1 = sbc("g1", [128, 2, D], F32)
                sc.dma("sp", g1[:], ln1[l].rearrange("a p d -> p a d"), writes=["g1"])
                wrt = sbc("wrt", [128, KC, 36], F32)
                sc.dma("sp", wrt[:], wr[l].rearrange("(k p) n -> p k n", p=128), writes=["wrt"])
                brt = sbc("brt", [1, 36], F32)
                sc.dma("sp", brt[:], br[l], writes=["brt"])
                hblk = [sbc(f"hblk{i}", [128, D], F32) for i in range(2)]
                hbfc = sbc("hbfc", [128, D], BF16)
                hTc = sbc("hTc", [128, KC, 128], BF16)
                ott = [sbc(f"ott{i}", [128, 4, 2, 512], BF16) for i in range(2)]
                merged = sbc("merged", [128, D], F32)
                gsb = [sbc(f"gsb{i}", [128, 512], F32) for i in range(2)]
                gtmp = [sbc(f"gtmp{i}", [128, 512], F32) for i in range(2)]
                mbf = sbc("mbf", [128, D], BF16)
                mT = sbc("mT", [128, KC, 128], BF16)
                rr = sbc("rr", [128, D], F32)
                h1s = [sbc(f"h1s{i}", [128, D], F32) for i in range(2)]
                h1hi = sbc("h1hi", [128, D], BF16)
                h1lo = sbc("h1lo", [128, D], BF16)
                h1Tl = sbc("h1Tl", [128, KC, 128], BF16)
                wrh = sbc("wrh", [128, KC, 36], BF16)
                wrl = sbc("wrl", [128, KC, 36], BF16)
                brh = sbc("brh", [1, 36], BF16)
                brl = sbc("brl", [1, 36], BF16)
                sc.op("dve", lambda e: e.tensor_copy(wrh[:], wrt[:]), reads=["wrt"], writes=["wrhl"])
                sc.op("dve", lambda e: e.tensor_tensor(wrl[:], wrt[:], wrh[:], ALU.subtract), reads=["wrt", "wrhl"], writes=["wrhl"])
                sc.op("dve", lambda e: e.tensor_copy(brh[:], brt[:]), reads=["brt"], writes=["wrhl"])
                sc.op("dve", lambda e: e.tensor_tensor(brl[:], brt[:], brh[:], ALU.subtract), reads=["brt", "wrhl"], writes=["wrhl"])
                h1Tb = [sbc(f"h1Tb{i}", [128, KC, 512], BF16) for i in range(2)]
                lgs = sbc("lgs", [128, 36], F32)
                rw = sbc("rw", [128, 16], F32)
                ohg = sbc("ohg", [128, 4], F32)
                sel = sbc("sel", [128, 8], F32)
                m8 = sbc("m8", [128, 8], F32)
                mk1 = sbc("mk1", [128, 8], F32)
                mk2 = sbc("mk2", [128, 8], F32)
                Gs = [sbc(f"Gs{i}", [128, NEXP], F32) for i in range(2)]
                gn = [0]
                for oi, b in enumerate(own):
                    i2 = oi % 2
                    hbk = ("hblk", i2)
                    sc.dma("sp", hblk[i2][:], hsrc[b * 128:(b + 1) * 128, :], reads=["hres", "src"], writes=[hbk])
                    og = (oi // 4) % 2
                    if oi % 4 == 0:
                        nt4 = min(512, NO - oi * 128)
                        for m_ in range(4):
                            sc.dma("act", ott[og][:, m_, :, 0:nt4], OT[m_, :, oi * 128:oi * 128 + nt4].rearrange("(c p) t -> p c t", p=128),
                                   reads=["OT"], writes=[("ott", og)])
                    osl_ = slice((oi % 4) * 128, (oi % 4) * 128 + 128)
                    sc.op("act", lambda e: e.activation(hbfc[:], hblk[i2][:], AF.Copy), reads=[hbk], writes=["hbfc"])
                    ptv = PS[7][:].bitcast(BF16)
                    for kc in range(KC):
                        sc.op("pe", lambda e, kc=kc: e.transpose(ptv[:, kc * 128:(kc + 1) * 128], hbfc[:, kc * 128:(kc + 1) * 128], id_b[:]),
                              reads=["hbfc", "id_b"], writes=pk(7))
                    sc.op("dve", lambda e: e.tensor_copy(hTc[:], ptv.rearrange("p (k t) -> p k t", k=KC)), reads=pk(7), writes=["hTc"])
                    for n in range(4):
                        for half in range(2):
                            hs = slice(half * 512, (half + 1) * 512)
                            pg = psget()
                            for kc in range(KC):
                                sc.op("pe", lambda e, kc=kc: e.matmul(PS[pg][:, :], lhsT=hTc[:, kc, :], rhs=wg[:, n, kc, hs], start=(kc == 0), stop=False),
                                      reads=["hTc", "wg"], writes=pk(pg))
                            sc.op("pe", lambda e: e.matmul(PS[pg][:, :], lhsT=ones_b[0:1, 0:128], rhs=bg[0:1, n, hs], start=False, stop=True),
                                  reads=["ones_b", "bg"], writes=pk(pg))
                            gi = gn[0] % 2; gn[0] += 1
                            sc.op("act", lambda e: e.activation(gsb[gi][:], PS[pg][:, :], AF.Sigmoid), reads=pk(pg), writes=[("gsb", gi)])
                            pp = psget()
                            for c in range(2):
                                sc.op("pe", lambda e, c=c: e.matmul(PS[pp][:, :], lhsT=ott[og][:, n, c, osl_], rhs=wb[:, n, c, hs], start=(c == 0), stop=(c == 1)),
                                      reads=[("ott", og), "wb"], writes=pk(pp))
                            if n == 0:
                                sc.op("dve", lambda e: e.tensor_tensor(merged[:, hs], gsb[gi][:], PS[pp][:, :], ALU.mult),
                                      reads=[("gsb", gi)] + pk(pp), writes=[("merged", half)])
                            else:
                                sc.op("dve", lambda e: e.tensor_tensor(gtmp[gi][:], gsb[gi][:], PS[pp][:, :], ALU.mult),
                                      reads=[("gsb", gi)] + pk(pp), writes=[("gtmp", gi)])
                                sc.op("pool", lambda e: e.tensor_tensor(merged[:, hs], merged[:, hs], gtmp[gi][:], ALU.add),
                                      reads=[("gtmp", gi), ("merged", half)], writes=[("merged", half)])
                    if CLVL < 2:
                        continue
                    sc.op("act", lambda e: e.activation(mbf[:], merged[:], AF.Copy), reads=[("merged", 0), ("merged", 1)], writes=["mbf"])
                    for kc in range(KC):
                        sc.op("pe", lambda e, kc=kc: e.transpose(ptv[:, kc * 128:(kc + 1) * 128], mbf[:, kc * 128:(kc + 1) * 128], id_b[:]),
                              reads=["mbf", "id_b"], writes=pk(7))
                    sc.op("dve", lambda e: e.tensor_copy(mT[:], ptv.rearrange("p (k t) -> p k t", k=KC)), reads=pk(7), writes=["mT"])
                    if CLVL < 2.1:
                        continue
                    pm = psget(2)
                    for half in range(2):
                        hs = slice(half * 512, (half + 1) * 512)
                        for kc in range(KC):
                            sc.op("pe", lambda e, kc=kc: e.matmul(PS[pm + half][:, :], lhsT=mT[:, kc, :], rhs=wo[:, kc, hs], start=(kc == 0), stop=(kc == KC - 1)),
                                  reads=["mT", "wo"], writes=pk(pm + half))
                        sc.op("dve", lambda e: e.scalar_tensor_tensor(rr[:, hs], hblk[i2][:, hs], ALPHA, PS[pm + half][:, :], ALU.mult, ALU.add),
                              reads=[hbk] + pk(pm + half), writes=["rr"])
                    if CLVL < 2.2:
                        continue
                    layer_norm(rr, "rr", g1, "g1", h1s[i2][:], ("h1s", i2), "ln1")
                    if CLVL < 2.3:
                        continue
                    sc.dma("sp", h1d[oi * 128:(oi + 1) * 128, :], h1s[i2][:], reads=[("h1s", i2)], writes=["h1d"])
                    if CLVL < 3:
                        continue
                    sc.op("act", lambda e: e.activation(h1hi[:], h1s[i2][:], AF.Copy), reads=[("h1s", i2)], writes=["h1hi"])
                    sc.op("dve", lambda e: e.tensor_tensor(h1lo[:], h1s[i2][:], h1hi[:], ALU.subtract), reads=[("h1s", i2), "h1hi"], writes=["h1lo"])
                    for kc in range(KC):
                        sc.op("pe", lambda e, kc=kc: e.transpose(ptv[:, kc * 128:(kc + 1) * 128], h1hi[:, kc * 128:(kc + 1) * 128], id_b[:]),
                              reads=["h1hi", "id_b"], writes=pk(7))
                    sc.op("dve", lambda e: e.tensor_copy(h1Tb[og][:, :, osl_], ptv.rearrange("p (k t) -> p k t", k=KC)), reads=pk(7), writes=[("h1Tb", og)])
                    for kc in range(KC):
                        sc.op("pe", lambda e, kc=kc: e.transpose(ptv[:, kc * 128:(kc + 1) * 128], h1lo[:, kc * 128:(kc + 1) * 128], id_b[:]),
                              reads=["h1lo", "id_b"], writes=pk(7))
                    sc.op("dve", lambda e: e.tensor_copy(h1Tl[:], ptv.rearrange("p (k t) -> p k t", k=KC)), reads=pk(7), writes=["h1Tl"])
                    if oi % 4 == 3 or oi == len(own) - 1:
                        o0_ = (oi // 4) * 512
                        n_ = (oi % 4 + 1) * 128
                        sc.dma("sp", h1T[:, o0_:o0_ + n_].rearrange("(k p) t -> p k t", p=128), h1Tb[og][:, :, 0:n_], reads=[("h1Tb", og)], writes=["h1T"])
                    if CLVL < 4:
                        continue
                    pr = psget()
                    first = True
                    for kc in range(KC):
                        for (lt, ltk, rt_) in ((h1Tb[og][:, kc, osl_], ("h1Tb", og), wrh), (h1Tb[og][:, kc, osl_], ("h1Tb", og), wrl), (h1Tl[:, kc, :], "h1Tl", wrh)):
                            sc.op("pe", lambda e, lt=lt, rt_=rt_, kc=kc, first=first: e.matmul(PS[pr][:, 0:36], lhsT=lt, rhs=rt_[:, kc, :], start=first, stop=False),
                                  reads=[ltk, "wrhl"], writes=pk(pr))
                            first = False
                    sc.op("pe", lambda e: e.matmul(PS[pr][:, 0:36], lhsT=ones_b[0:1, 0:128], rhs=brh[0:1, :], start=False, stop=False),
                          reads=["ones_b", "wrhl"], writes=pk(pr))
                    sc.op("pe", lambda e: e.matmul(PS[pr][:, 0:36], lhsT=ones_b[0:1, 0:128], rhs=brl[0:1, :], start=False, stop=True),
                          reads=["ones_b", "wrhl"], writes=pk(pr))
                    sc.op("act", lambda e: e.activation(lgs[:], PS[pr][:, 0:36], AF.Copy), reads=pk(pr), writes=["lgs"])
                    if CLVL < 5:
                        continue
                    V = sc.op
                    V("dve", lambda e: e.tensor_reduce(rw[:, 0:1], lgs[:, 0:4], AX.X, ALU.max), reads=["lgs"], writes=["rw0"])
                    V("dve", lambda e: e.tensor_scalar(rw[:, 1:2], rw[:, 0:1], -1.0, None, ALU.mult), reads=["rw0"], writes=["rw1"])
                    V("act", lambda e: e.activation(ohg[:], lgs[:, 0:4], AF.Exp, bias=rw[:, 1:2], scale=1.0, accum_out=rw[:, 2:3]),
                      reads=["lgs", "rw1"], writes=["ohg", "rw2"])
                    V("dve", lambda e: e.reciprocal(rw[:, 3:4], rw[:, 2:3]), reads=["rw2"], writes=["rw3"])
                    V("dve", lambda e: e.tensor_scalar(ohg[:], lgs[:, 0:4], rw[:, 0:1], None, ALU.is_equal), reads=["lgs", "rw0", "ohg"], writes=["ohg"])
                    V("dve", lambda e: e.tensor_scalar(sel[:], lgs[:, 4:12], ohg[:, 0:1], None, ALU.mult), reads=["lgs", "ohg"], writes=["sel"])
                    for g_ in range(1, 4):
                        V("dve", lambda e, g_=g_: e.scalar_tensor_tensor(sel[:], lgs[:, 4 + 8 * g_:12 + 8 * g_], ohg[:, g_:g_ + 1], sel[:], ALU.mult, ALU.add),
                          reads=["lgs", "ohg", "sel"], writes=["sel"])
                    V("dve", lambda e: e.max(m8[:], sel[:]), reads=["sel"], writes=["m8"])
                    V("dve", lambda e: e.tensor_tensor(rw[:, 4:5], m8[:, 1:2], m8[:, 0:1], ALU.subtract), reads=["m8"], writes=["rw4"])
                    V("act", lambda e: e.activation(rw[:, 5:6], rw[:, 4:5], AF.Exp), reads=["rw4"], writes=["rw5"])
                    V("dve", lambda e: e.tensor_scalar(rw[:, 6:7], rw[:, 5:6], 1.0, None, ALU.add), reads=["rw5"], writes=["rw6"])
                    V("dve", lambda e: e.reciprocal(rw[:, 6:7], rw[:, 6:7]), reads=["rw6"], writes=["rw6"])
                    V("dve", lambda e: e.tensor_tensor(rw[:, 7:8], rw[:, 6:7], rw[:, 3:4], ALU.mult), reads=["rw6", "rw3"], writes=["rw7"])
                    V("dve", lambda e: e.tensor_tensor(rw[:, 8:9], rw[:, 7:8], rw[:, 5:6], ALU.mult), reads=["rw7", "rw5"], writes=["rw8"])
                    V("dve", lambda e: e.tensor_scalar(mk1[:], sel[:], m8[:, 0:1], rw[:, 7:8], ALU.is_equal, ALU.mult), reads=["sel", "m8", "rw7"], writes=["mk1"])
                    V("dve", lambda e: e.tensor_scalar(mk2[:], sel[:], m8[:, 1:2], rw[:, 8:9], ALU.is_equal, ALU.mult), reads=["sel", "m8", "rw8"], writes=["mk2"])
                    V("dve", lambda e: e.tensor_tensor(mk1[:], mk1[:], mk2[:], ALU.add), reads=["mk1", "mk2"], writes=["mk1"])
                    Gt_ = Gs[i2]
                    for g_ in range(4):
                        V("dve", lambda e, g_=g_: e.tensor_scalar(Gt_[:, 8 * g_:8 * g_ + 8], mk1[:], ohg[:, g_:g_ + 1], None, ALU.mult),
                          reads=["mk1", "ohg"], writes=[("Gs", i2)])
                    sc.dma("sp", Gd[oi * 128:(oi + 1) * 128, :], Gt_[:], reads=[("Gs", i2)], writes=["Gd"])
                if PHASES == 3:
                    sc.dma("sp", rr[:], h1d[0:128, :], reads=["h1d"], writes=["rr"])
                    sc.dma("sp", h1Tb[0][:, :, 0:128], h1T[:, 0:128].rearrange("(k p) t -> p k t", p=128), reads=["h1T"], writes=[("h1Tb", 0)])
                    sc.dma("sp", Gs[0][:], Gd[0:128, :], reads=["Gd"], writes=[("Gs", 0)])
                sc.barrier()
            if PHASES < 4:
                continue
            with contextlib.ExitStack() as pdx:
                def sbd(name, shape, dt):
                    return pdx.enter_context(nc.sbuf_tensor(f"D{l}_{name}", list(shape), dt))
                SB_ = min(16, len(own))
                Y = sbd("Y", [128, SB_, D], F32)
                xT = sbd("xT", [128, KC, SB_ * 128], BF16)
                Gt = sbd("Gt", [128, SB_, NEXP], F32)
                w1t = [sbd(f"w1t{i}", [128, KC, DE], BF16) for i in range(2)]
                w3t = [sbd(f"w3t{i}", [128, KC, DE], BF16) for i in range(2)]
                w2t = [sbd(f"w2t{i}", [128, 4, D], BF16) for i in range(2)]
                sil = [sbd(f"sil{i}", [128, 512], F32) for i in range(2)]
                hTe = [sbd(f"hTe{i}", [128, 4, 512], BF16) for i in range(2)]
                rr2 = sbd("rr2", [128, D], F32)
                g2 = sbd("g2", [128, 2, D], F32)
                sc.dma("sp", g2[:], ln2[l].rearrange("a p d -> p a d"), writes=["g2"])
                hh = [sbd(f"hh{i}", [128, D], F32) for i in range(2)]
                oo = [sbd(f"oo{i}", [128, D], F32) for i in range(2)]
                sn = [0]; hn = [0]
                pend = [None]

                def emit_w2(ex, wi, hi, t0, nbt):
                    for j in range(nbt):
                        for half in range(2):
                            hs = slice(half * 512, (half + 1) * 512)
                            py = psget()
                            for c in range(4):
                                sc.op("pe", lambda e, c=c: e.matmul(PS[py][:, :], lhsT=hTe[hi][:, c, j * 128:(j + 1) * 128], rhs=w2t[wi][:, c, hs],
                                                                     start=(c == 0), stop=(c == 3)),
                                      reads=[("hTe", hi), ("w2t", wi)], writes=pk(py))
                            yk = ("Y", t0 + j, half)
                            if ex == 0:
                                sc.op("dve", lambda e: e.tensor_scalar(Y[:, t0 + j, hs], PS[py][:, :], Gt[:, t0 + j, ex:ex + 1], None, ALU.mult),
                                      reads=pk(py) + ["Gt"], writes=[yk])
                            else:
                                sc.op("dve", lambda e: e.scalar_tensor_tensor(Y[:, t0 + j, hs], PS[py][:, :], Gt[:, t0 + j, ex:ex + 1], Y[:, t0 + j, hs],
                                                                              ALU.mult, ALU.add),
                                      reads=pk(py) + ["Gt", yk], writes=[yk])

                for s0 in range(0, len(own), SB_):
                    nbs = min(SB_, len(own) - s0)
                    ntok = nbs * 128
                    sc.dma("sp", xT[:, :, 0:ntok], h1T[:, s0 * 128:s0 * 128 + ntok].rearrange("(k p) t -> p k t", p=128), reads=["h1T"], writes=["xT"])
                    sc.dma("sp", Gt[:, 0:nbs, :], Gd[s0 * 128:s0 * 128 + ntok, :].rearrange("(b p) e -> p b e", p=128), reads=["Gd"], writes=["Gt"])
                    for ex in range(NEXP):
                        wi = ex % 2
                        sc.dma("pool", w1t[wi][:], ew1[l, ex].rearrange("(k p) f -> p k f", p=128), writes=[("w1t", wi)])
                        sc.dma("pool", w3t[wi][:], ew3[l, ex].rearrange("(k p) f -> p k f", p=128), writes=[("w3t", wi)])
                        sc.dma("pool", w2t[wi][:], ew2[l, ex].rearrange("(c p) d -> p c d", p=128), writes=[("w2t", wi)])
                        for t0 in range(0, nbs, 4):
                            nbt = min(4, nbs - t0)
                            nt = nbt * 128
                            tcs = slice(t0 * 128, t0 * 128 + nt)
                            hi = hn[0] % 2; hn[0] += 1
                            for c in range(4):
                                fs = slice(c * 128, (c + 1) * 128)
                                pa_ = psget(); pb_ = psget()
                                for kc in range(KC):
                                    sc.op("pe", lambda e, kc=kc: e.matmul(PS[pa_][:, 0:nt], lhsT=w1t[wi][:, kc, fs], rhs=xT[:, kc, tcs], start=(kc == 0), stop=(kc == KC - 1)),
                                          reads=[("w1t", wi), "xT"], writes=pk(pa_))
                                for kc in range(KC):
                                    sc.op("pe", lambda e, kc=kc: e.matmul(PS[pb_][:, 0:nt], lhsT=w3t[wi][:, kc, fs], rhs=xT[:, kc, tcs], start=(kc == 0), stop=(kc == KC - 1)),
                                          reads=[("w3t", wi), "xT"], writes=pk(pb_))
                                si = sn[0] % 2; sn[0] += 1
                                sc.op("act", lambda e: e.activation(sil[si][:, 0:nt], PS[pa_][:, 0:nt], AF.Silu), reads=pk(pa_), writes=[("sil", si)])
                                sc.op("dve", lambda e: e.tensor_tensor(hTe[hi][:, c, 0:nt], sil[si][:, 0:nt], PS[pb_][:, 0:nt], ALU.mult),
                                      reads=[("sil", si)] + pk(pb_), writes=[("hTe", hi)])
                            if pend[0] is not None:
                                emit_w2(*pend[0])
                            pend[0] = (ex, wi, hi, t0, nbt)
                    if pend[0] is not None:
                        emit_w2(*pend[0])
                        pend[0] = None
                    for j in range(nbs):
                        oi = s0 + j
                        i2 = oi % 2
                        sc.dma("sp", hh[i2][:], h1d[oi * 128:(oi + 1) * 128, :], reads=["h1d"], writes=[("hh", i2)])
                        sc.op("dve", lambda e: e.scalar_tensor_tensor(rr2[:], hh[i2][:], ALPHA, Y[:, j, :], ALU.mult, ALU.add),
                              reads=[("hh", i2), ("Y", j, 0), ("Y", j, 1)], writes=["rr2"])
                        layer_norm(rr2, "rr2", g2, "g2", oo[i2][:], ("oo", i2), "ln2")
                        sc.dma("sp", dst[oi * 128:(oi + 1) * 128, :], oo[i2][:], reads=[("oo", i2)], writes=["dst" if last else "src"])
                sc.barrier()
        sc.finish()
    return nc, sc


PHASES = 4
DBG_MIX = MIX
CLVL = 9
BQ = 'act'


def _t5_bucket_np(rel):
    import jax, jax.numpy as jnp
    cpu = jax.devices("cpu")[0]
    with jax.default_device(cpu):
        rel = jnp.asarray(rel, dtype=jnp.int32)
        nb = 16
        max_exact = 8
        ret = jnp.where(rel > 0, nb, 0)
        n = jnp.abs(rel)
        nf = jnp.maximum(n, 1).astype(jnp.float32)
        large = max_exact + (jnp.log(nf / max_exact) / math.log(128 / max_exact) * (nb - max_exact)).astype(jnp.int32)
        large = jnp.minimum(large, nb - 1)
        return np.asarray(ret + jnp.where(n < max_exact, n, large))


def prep_weights(inp, S):
    f = lambda a: np.ascontiguousarray(np.asarray(a, dtype=np.float32))
    L = DEPTH
    w_in = f(inp["w_in"])
    r = np.arange
    cols = np.concatenate([r(256, 512), r(1024, 1280), r(2208, 2464), r(1792, 1920), r(1920, 1952),
                           r(1936, 1952), r(1920, 1936), r(2720, 2724),
                           r(0, 256), r(768, 1024), r(1952, 2208), r(1536, 1792),
                           r(512, 768), r(1280, 1536), r(2464, 2720)])
    assert len(cols) == WEXT
    d = {}
    d["w_ext"] = f(w_in[:, :, cols])
    d["lnin"] = f(np.stack([np.broadcast_to(inp["ln_in_g"], (128, D)), np.broadcast_to(inp["ln_in_b"], (128, D))]))
    d["nbf"] = f(np.asarray(inp["b_forget"]).reshape(L, 4, 1))
    d["dlam"] = f(np.broadcast_to(np.asarray(inp["diff_lambda"]).reshape(L, 1, 128), (L, 128, 128)))
    d["dng"] = f(np.asarray(inp["diff_norm_g"]).reshape(L, 64, 1))
    kq = r(128)
    rel0 = kq[:, None] - kq[None, :]
    rel1 = rel0 - 128
    t5 = f(inp["t5_table"])
    b0 = _t5_bucket_np(rel0); b1 = _t5_bucket_np(rel1)
    d["tbias"] = f(np.stack([np.stack([t5[b0][:, :, h], t5[b1][:, :, h]]) for h in range(4)]))
    d["tconst"] = f(np.broadcast_to(t5[15][None, :], (128, 4)))
    crb = f(inp["chunk_rel_bias"])
    i0 = np.clip(rel0, -128, 128) + 128; i1 = np.clip(rel1, -128, 128) + 128
    d["cbias"] = f(np.stack([np.stack([np.stack([crb[l][i0][:, :, h], crb[l][i1][:, :, h]]) for h in range(4)]) for l in range(L)]))
    d["cconst"] = f(np.stack([np.broadcast_to(crb[l][0][None, :], (128, 4)) for l in range(L)]))
    d["mqg"] = f(np.asarray(inp["mla_q_norm_g"]).reshape(L, 256, 1))
    d["mkvg"] = f(np.asarray(inp["mla_kv_norm_g"]).reshape(L, 128, 1))
    wuq = f(inp["mla_w_uq"])
    ucols = []
    for h in range(4):
        ucols += list(r(96 * h, 96 * h + 96)) + list(r(96 * h, 96 * h + 64)) + list(r(96 * h + 80, 96 * h + 96)) + list(r(96 * h + 64, 96 * h + 80))
    d["w_uq"] = f(wuq[:, :, np.array(ucols)])
    wukv = f(inp["mla_w_ukv"])
    kc_ = np.concatenate([r(128 * h, 128 * h + 64) for h in range(4)] + [r(128 * h + 64, 128 * h + 128) for h in range(4)])
    d["w_ukv"] = f(wukv[:, :, kc_])
    d["ropet"] = rope_table(S, 0)
    d["w_gate"] = f(inp["w_gate"]); d["b_gate"] = f(inp["b_gate"]); d["w_branch"] = f(inp["w_branch"]); d["w_out"] = f(inp["w_out"])
    d["ln1"] = f(np.stack([np.stack([np.broadcast_to(inp["ln1_g"][l], (128, D)), np.broadcast_to(inp["ln1_b"][l], (128, D))]) for l in range(L)]))
    d["ln2"] = f(np.stack([np.stack([np.broadcast_to(inp["ln2_g"][l], (128, D)), np.broadcast_to(inp["ln2_b"][l], (128, D))]) for l in range(L)]))
    d["wr"] = f(np.concatenate([inp["router_group_w"], inp["router_expert_w"]], axis=2))
    d["br"] = f(np.concatenate([inp["router_group_b"], inp["router_expert_b"]], axis=1).reshape(L, 1, 36))
    d["ew1"] = f(inp["expert_w1"]); d["ew3"] = f(inp["expert_w3"]); d["ew2"] = f(inp["expert_w2"])
    d["ident"] = np.eye(128, dtype=np.float32)
    d["trimask"] = (kq[:, None] <= kq[None, :]).astype(np.float32)
    return d


def rope_table(S, pos0):
    inv_freq = np.power(np.float32(10000.0), -np.arange(16, dtype=np.float32) * np.float32(2.0) / np.float32(32))
    pos = np.maximum(np.arange(S, dtype=np.float32) + np.float32(pos0), np.float32(0))
    ang = pos[:, None] * inv_freq[None, :].astype(np.float32)
    cs, sn = np.cos(ang).astype(np.float32).T, np.sin(ang).astype(np.float32).T
    return np.ascontiguousarray(np.stack([np.concatenate([cs, cs]), np.concatenate([-sn, sn])]).astype(np.float32))


def make_in_maps(inputs, S, nbatch):
    w = prep_weights(inputs, S)
    x = np.asarray(inputs["x"], dtype=np.float32)
    rope0 = rope_table(S, -128)
    maps = []
    for c in range(2 * nbatch):
        b, p = c // 2, c % 2
        m = dict(w)
        if p == 1:
            m["xin"] = np.ascontiguousarray(x[b, :S])
            m["keep"] = np.ones((128, 1), np.float32)
        else:
            m["xin"] = np.ascontiguousarray(np.concatenate([np.zeros((128, D), np.float32), x[b, :S - 128]], axis=0))
            m["keep"] = np.zeros((128, 1), np.float32)
            m["ropet"] = rope0
        maps.append(m)
    return maps


def assemble(results, S, nbatch):
    out = np.empty((nbatch, S, D), np.float32)
    nown = S // 256
    for c in range(2 * nbatch):
        b, p = c // 2, c % 2
        o = np.asarray(results[c]["out"], dtype=np.float32).reshape(nown, 128, D)
        ov = out[b].reshape(S // 128, 128, D)
        if p == 1:
            ov[1::2] = o
        else:
            ov[0::2] = o
    return out


def own_config(S):
    nb = S // 128
    return {0: list(range(nb)), 1: list(range(1, nb, 2))}


def kernel(**inputs):
    S, nbatch = 8192, 4
    nc, _ = build_program(S, [0, 1], own_config(S))
    maps = make_in_maps(inputs, S, nbatch)
    res = run_bass_kernel_spmd(nc, maps, core_ids=list(range(2 * nbatch)))
    return assemble(res.results, S, nbatch)
```
